# Optimizing a Trainium2 kernel written in Bass

```python
import math
import jax, jax.numpy as jnp
from jax import lax
import numpy as np

D_MODEL = 1024
BATCH = 8
SEQ = 4096
DEPTH = 1

MIX_WIDTH = D_MODEL
ATT_HEAD_DIM = 64
ATT_HEADS = (MIX_WIDTH // 2) // ATT_HEAD_DIM
ATT_KV_HEADS = max(1, ATT_HEADS // 4)
ATT_GROUP = ATT_HEADS // ATT_KV_HEADS
ATT_SCALE = ATT_HEAD_DIM ** -0.5
WINDOW = 128
BLOCK = WINDOW
N_BUCKETS = 32
MAX_DISTANCE = 128
HG_DK = 128
HG_DV = 128
HG_HEADS = (MIX_WIDTH // 2) // HG_DV
CHUNK = 64
N_EXPERTS = 32
TOP_K = 4
D_FF = D_MODEL
SWIGLU_LIMIT = 7.0
SWIGLU_ALPHA = 1.702
EPS = 1e-5

ATT_Q = ATT_HEADS * ATT_HEAD_DIM
ATT_KV = ATT_KV_HEADS * ATT_HEAD_DIM
HG_K = HG_HEADS * HG_DK
HG_V = HG_HEADS * HG_DV
OUT_IN = ATT_Q + HG_V
IN_SIZES = (ATT_Q, ATT_KV, ATT_KV, HG_K, HG_K, HG_V, HG_V)
IN_COLS = sum(IN_SIZES)
IN_SPLITS = tuple(sum(IN_SIZES[:j + 1]) for j in range(len(IN_SIZES) - 1))

kernel_name = 'hymba_swa_sink_hgrn2_moe_adaln'


def rms_norm(x, g):
    xf = x.astype(jnp.float32)
    y = xf * lax.rsqrt(jnp.mean(xf * xf, axis=-1, keepdims=True) + EPS)
    return (y * g.astype(jnp.float32)).astype(x.dtype)


def t5_causal_bucket(dist):
    n = jnp.maximum(dist, 0)
    max_exact = N_BUCKETS // 2
    nf = jnp.maximum(n, 1).astype(jnp.float32)
    large = max_exact + (jnp.log(nf / max_exact) / math.log(MAX_DISTANCE / max_exact)
                         * (N_BUCKETS - max_exact)).astype(jnp.int32)
    large = jnp.minimum(large, N_BUCKETS - 1)
    return jnp.where(n < max_exact, n, large)


def band_structure(rel_bias, n_blocks):
    i = jnp.arange(BLOCK, dtype=jnp.int32)[:, None]
    m = jnp.arange(2 * BLOCK, dtype=jnp.int32)[None, :]
    dist = i + BLOCK - m
    in_window = (dist >= 0) & (dist < WINDOW)
    bias = rel_bias.astype(jnp.float32)[t5_causal_bucket(dist)]
    bias = jnp.transpose(bias, (2, 0, 1)).reshape(ATT_KV_HEADS, ATT_GROUP, BLOCK, 2 * BLOCK)
    blk = jnp.arange(n_blocks, dtype=jnp.int32)[:, None, None]
    key_exists = (blk > 0) | (m[None] >= BLOCK)
    valid = in_window[None] & key_exists
    return bias, valid


def sliding_window_attention(q, k, v, sinks, band_bias, valid):
    B, S = q.shape[0], q.shape[1]
    nb = S // BLOCK
    qb = q.reshape(B, nb, BLOCK, ATT_KV_HEADS, ATT_GROUP, ATT_HEAD_DIM)

    def band(t):
        prev = jnp.pad(t, ((0, 0), (BLOCK, 0), (0, 0), (0, 0)))[:, :S]
        prev = prev.reshape(B, nb, BLOCK, ATT_KV_HEADS, ATT_HEAD_DIM)
        cur = t.reshape(B, nb, BLOCK, ATT_KV_HEADS, ATT_HEAD_DIM)
        return jnp.concatenate([prev, cur], axis=2)

    kb, vb = band(k), band(v)
    s = jnp.einsum('bnqkgd,bnskd->bnkgqs', qb, kb).astype(jnp.float32) * ATT_SCALE + band_bias
    s = jnp.where(valid[None, :, None, None], s, -jnp.inf)
    sink = jnp.broadcast_to(sinks.astype(jnp.float32).reshape(ATT_KV_HEADS, ATT_GROUP, 1, 1),
                            s.shape[:-1] + (1,))
    p = jax.nn.softmax(jnp.concatenate([s, sink], axis=-1), axis=-1)[..., :-1]
    o = jnp.einsum('bnkgqs,bnskd->bnqkgd', p.astype(v.dtype), vb)
    return o.reshape(B, S, ATT_Q)


def hgrn2_chunkwise(q, f_logit, inp, lb):
    B, S, H, DK = q.shape
    DV = inp.shape[-1]
    nc = S // CHUNK
    f = lb + (1.0 - lb) * jax.nn.sigmoid(f_logit.astype(jnp.float32))
    log_f = jnp.log(f)
    kk = 1.0 - f
    qf = jax.nn.silu(q.astype(jnp.float32))

    def to_chunks(t):
        return t.reshape(B, nc, CHUNK, H, t.shape[-1]).transpose(1, 0, 3, 2, 4)

    causal = jnp.tril(jnp.ones((CHUNK, CHUNK), dtype=bool))[:, :, None]

    def step(state, xs):
        qc, kc, vc, gc = xs
        b = jnp.cumsum(gc, axis=2)
        b_end = b[:, :, -1:, :]
        decay = jnp.exp(jnp.where(causal, b[:, :, :, None, :] - b[:, :, None, :, :], -jnp.inf))
        scores = jnp.einsum('bhtk,bhsk,bhtsk->bhts', qc, kc, decay)
        o = (jnp.einsum('bhts,bhsv->bhtv', scores, vc)
             + jnp.einsum('bhtk,bhkv->bhtv', qc * jnp.exp(b), state))
        state = (jnp.exp(b_end[:, :, 0, :, None]) * state
                 + jnp.einsum('bhsk,bhsv->bhkv', kc * jnp.exp(b_end - b), vc))
        return state, o

    state0 = jnp.zeros((B, H, DK, DV), jnp.float32)
    _, o = lax.scan(step, state0, (to_chunks(qf), to_chunks(kk),
                                   to_chunks(inp.astype(jnp.float32)), to_chunks(log_f)))
    return o.transpose(1, 0, 3, 2, 4).reshape(B, S, H, DV).astype(inp.dtype)


def moe_ffn(h, w_router, b_router, w_gate_up, b_gate_up, w_down, b_down):
    B, S, D = h.shape
    t = h.reshape(B * S, D)
    logits = (t @ w_router + b_router).astype(jnp.float32)
    top_val, top_idx = lax.top_k(logits, TOP_K)
    top_w = jax.nn.softmax(top_val, axis=-1)
    combine = jnp.sum(jax.nn.one_hot(top_idx, N_EXPERTS, dtype=jnp.float32) * top_w[..., None], axis=1)
    out = jnp.zeros((B * S, D), jnp.float32)
    for e in range(N_EXPERTS):
        gu = t @ w_gate_up[e] + b_gate_up[e]
        gate = jnp.minimum(gu[:, 0::2], SWIGLU_LIMIT)
        up = jnp.clip(gu[:, 1::2], -SWIGLU_LIMIT, SWIGLU_LIMIT)
        y = ((up + 1.0) * gate * jax.nn.sigmoid(SWIGLU_ALPHA * gate)) @ w_down[e] + b_down[e]
        out = out + combine[:, e:e + 1] * y.astype(jnp.float32)
    return out.astype(h.dtype).reshape(B, S, D)


def setup_inputs(seed: int = 0) -> dict:
    key = jax.random.key(seed)
    ks = jax.random.split(key, 24)
    f32 = jnp.float32
    L = DEPTH

    def nrm(k, shape, scale):
        return scale * jax.random.normal(k, shape, f32)

    return {
        'x': nrm(ks[0], (BATCH, SEQ, D_MODEL), 1.0),
        'c': nrm(ks[1], (BATCH, D_MODEL), 1.0),
        'w_ada': nrm(ks[2], (L, D_MODEL, 6 * D_MODEL), 0.5 * D_MODEL ** -0.5),
        'b_ada': nrm(ks[3], (L, 6 * D_MODEL), 0.02),
        'g_mix': 1.0 + nrm(ks[4], (L, D_MODEL), 0.02),
        'w_in': nrm(ks[5], (L, D_MODEL, IN_COLS), D_MODEL ** -0.5),
        'b_in': nrm(ks[6], (L, IN_COLS), 0.02),
        'attn_sinks': nrm(ks[7], (L, ATT_HEADS), 0.5),
        'rel_bias': nrm(ks[8], (N_BUCKETS, ATT_HEADS), 0.5),
        'hg_lb': nrm(ks[9], (L + 1, HG_K), 1.0),
        'hg_norm_w': 1.0 + nrm(ks[10], (L, HG_DV), 0.02),
        'w_out': nrm(ks[11], (L, OUT_IN, D_MODEL), OUT_IN ** -0.5),
        'b_out': nrm(ks[12], (L, D_MODEL), 0.02),
        'g_ffn': 1.0 + nrm(ks[13], (L, D_MODEL), 0.02),
        'w_router': nrm(ks[14], (L, D_MODEL, N_EXPERTS), D_MODEL ** -0.5),
        'b_router': nrm(ks[15], (L, N_EXPERTS), 0.01),
        'w_gate_up': nrm(ks[16], (L, N_EXPERTS, D_MODEL, 2 * D_FF), D_MODEL ** -0.5),
        'b_gate_up': nrm(ks[17], (L, N_EXPERTS, 2 * D_FF), 0.02),
        'w_down': nrm(ks[18], (L, N_EXPERTS, D_FF, D_MODEL), D_FF ** -0.5),
        'b_down': nrm(ks[19], (L, N_EXPERTS, D_MODEL), 0.02),
        'g_final': 1.0 + nrm(ks[20], (D_MODEL,), 0.02),
    }


def reference(x, c, w_ada, b_ada, g_mix, w_in, b_in, attn_sinks, rel_bias, hg_lb, hg_norm_w,
              w_out, b_out, g_ffn, w_router, b_router, w_gate_up, b_gate_up, w_down, b_down,
              g_final):
    B, S, D = x.shape
    band_bias, valid = band_structure(rel_bias, S // BLOCK)
    lb_all = jnp.cumsum(jax.nn.softmax(hg_lb.astype(jnp.float32), axis=0), axis=0)
    cond = jax.nn.silu(c)
    for l in range(DEPTH):
        mod = (cond @ w_ada[l] + b_ada[l]).reshape(B, 6, 1, D)
        sh1, sc1, gt1, sh2, sc2, gt2 = (mod[:, j] for j in range(6))

        h = rms_norm(x, g_mix[l]) * (1.0 + sc1) + sh1
        proj = h @ w_in[l] + b_in[l]
        aq, ak, av, hq, hf, hi, hg = jnp.split(proj, IN_SPLITS, axis=-1)
        y_att = sliding_window_attention(
            aq.reshape(B, S, ATT_HEADS, ATT_HEAD_DIM),
            ak.reshape(B, S, ATT_KV_HEADS, ATT_HEAD_DIM),
            av.reshape(B, S, ATT_KV_HEADS, ATT_HEAD_DIM),
            attn_sinks[l], band_bias, valid)
        o_hg = hgrn2_chunkwise(
            hq.reshape(B, S, HG_HEADS, HG_DK),
            hf.reshape(B, S, HG_HEADS, HG_DK),
            hi.reshape(B, S, HG_HEADS, HG_DV),
            lb_all[l].reshape(HG_HEADS, HG_DK))
        o_hg = rms_norm(o_hg, hg_norm_w[l]) * jax.nn.silu(hg.reshape(B, S, HG_HEADS, HG_DV))
        mixed = jnp.concatenate([y_att, o_hg.reshape(B, S, HG_V)], axis=-1)
        x = x + gt1 * (mixed @ w_out[l] + b_out[l])

        h = rms_norm(x, g_ffn[l]) * (1.0 + sc2) + sh2
        x = x + gt2 * moe_ffn(h, w_router[l], b_router[l], w_gate_up[l], b_gate_up[l],
                              w_down[l], b_down[l])
    return rms_norm(x, g_final)
```

```python
import contextlib
import math

import numpy as np
import concourse.bass as bass
import concourse.mybir as mybir
from concourse.bass_utils import run_bass_kernel_spmd

F32 = mybir.dt.float32
BF16 = mybir.dt.bfloat16
AF = mybir.ActivationFunctionType
ALU = mybir.AluOpType
AX = mybir.AxisListType

D = 1024
NE = 32
EPS = 1e-5
ATT_SCALE = 64 ** -0.5
IN_COLS = 2816
NEG = -30000.0

PV_C, PV_GMIX, PV_GFFN, PV_BADA, PV_BIN, PV_BQ, PV_LB, PV_NW, PV_SINK, PV_BGU = 0, 8, 16, 24, 72, 94, 98, 106, 107, 111
NPV = PV_BGU + 512


class _Op:
    __slots__ = ("stream", "fn", "dma", "deps", "sig", "tok")


class Sched:
    STREAMS = ("pe", "act", "dve", "pool", "sp")

    def __init__(self):
        self.ops = {s: [] for s in self.STREAMS}
        self.last_write = {}
        self.readers = {}
        self.dma_counts = {}

    def add(self, stream, fn, r=(), w=(), dma=None):
        op = _Op()
        op.stream, op.fn, op.dma, op.sig, op.tok = stream, fn, dma is not None, False, None
        pr = [x for x in r if x.startswith("pb")]
        if pr:
            r = [x for x in r if not x.startswith("pb")]
            w = list(w) + pr
        deps = {}
        for x in r:
            d = self.last_write.get(x)
            if d is not None:
                deps[id(d)] = (d, True)
        for x in w:
            d = self.last_write.get(x)
            if d is not None and id(d) not in deps:
                deps[id(d)] = (d, False)
            for rd in self.readers.get(x, ()):
                if id(rd) not in deps:
                    deps[id(rd)] = (rd, False)
        op.deps = []
        for d, raw in deps.values():
            if d.dma or d.stream != stream or op.dma:
                need = True
            else:
                need = stream != "pe"
            if need:
                op.deps.append(d)
                d.sig = True
        for x in r:
            self.readers.setdefault(x, []).append(op)
        for x in w:
            self.last_write[x] = op
            self.readers[x] = []
        if op.dma:
            c = self.dma_counts.get(dma, 0) + 16
            self.dma_counts[dma] = c
            op.tok = (("dma", dma), c)
        self.ops[stream].append(op)
        return op

    def emit(self, nc):
        for s in self.STREAMS:
            c = 0
            for op in self.ops[s]:
                if not op.dma and op.sig:
                    c += 1
                    op.tok = (("eng", s), c)
        keys = [("eng", s) for s in self.STREAMS] + [("dma", k) for k in self.dma_counts]
        with contextlib.ExitStack() as es:
            sems = {}
            for k in keys:
                sems[k] = es.enter_context(nc.semaphore("s_%s_%s" % (k[0], k[1])))
            block = es.enter_context(nc.Block())
            sched = self

            def run_stream(s, eng):
                waited = {}
                for op in sched.ops[s]:
                    need = {}
                    for d in op.deps:
                        k, v = d.tok
                        if k[0] == "dma" and k[1].startswith("T:"):
                            v = sched.dma_counts[k[1]]
                        if need.get(k, 0) < v:
                            need[k] = v
                    for k, v in need.items():
                        if waited.get(k, 0) < v:
                            eng.wait_ge(sems[k], v)
                            waited[k] = v
                    ins = op.fn(eng)
                    if op.dma:
                        ins.then_inc(sems[op.tok[0]], 16)
                    elif op.sig:
                        ins.then_inc(sems[op.tok[0]], 1)
                if s == "sp":
                    for k, c in sched.dma_counts.items():
                        if waited.get(("dma", k), 0) < c:
                            eng.wait_ge(sems[("dma", k)], c)

            @block.tensor
            def _(e):
                run_stream("pe", e)

            @block.scalar
            def _(e):
                run_stream("act", e)

            @block.vector
            def _(e):
                run_stream("dve", e)

            @block.gpsimd
            def _(e):
                run_stream("pool", e)

            @block.sync
            def _(e):
                run_stream("sp", e)


def build(SEQ=4096, dbg=()):
    nc = bass.Bass("TRN2", target_bir_lowering=False)
    NGRP = SEQ // 1024

    def din(name, shape):
        return nc.dram_tensor(name, list(shape), F32, kind="ExternalInput").ap()

    x = din("x", [SEQ, D])
    pvec = din("pvec", [128, NPV])
    w_ada = din("w_ada", [D, 6 * D])
    w_in = din("w_in", [D, IN_COLS])
    rowsin = din("rowsin", [2, D])
    relb = din("relb", [32, 8])
    ohb = din("ohb", [32, 510])
    negb = din("negb", [8, 510])
    cst = din("cst", [128, 640])
    w_out = din("w_out", [D, D])
    w_router = din("w_router", [D, NE])
    brt = din("brt", [1, NE])
    w_gu = din("w_gu", [NE, D, 2 * D])
    w_dn = din("w_dn", [NE, D, D])
    b_dn = din("b_dn", [NE, D])
    gfin = din("gfin", [1, D])
    out = nc.dram_tensor("out", [SEQ, D], F32, kind="ExternalOutput").ap()
    scr = nc.dram_tensor("scr", [8, 2, 129 * 255], F32, kind="Internal").ap()
    dbg_out = {}
    if "x1" in dbg:
        dbg_out["x1"] = nc.dram_tensor("dbg_x1", [SEQ, D], F32, kind="ExternalOutput").ap()
    if "comb" in dbg:
        dbg_out["comb"] = nc.dram_tensor("dbg_comb", [SEQ, NE], F32, kind="ExternalOutput").ap()
    if "mix" in dbg:
        dbg_out["aT"] = nc.dram_tensor("dbg_aT", [SEQ // 512, 128, 4, 512], BF16, kind="ExternalOutput").ap()
        dbg_out["mTh"] = nc.dram_tensor("dbg_mTh", [SEQ // 512, 128, 4, 512], BF16, kind="ExternalOutput").ap()

    S = Sched()
    lane_stack = []

    def A(stream, fn, r=(), w=(), dma=None):
        if lane_stack:
            lane_stack[-1].append((stream, fn, tuple(r), tuple(w), dma))
        else:
            S.add(stream, fn, r=r, w=w, dma=dma)

    def record(f, *args):
        lane_stack.append([])
        f(*args)
        return lane_stack.pop()

    def weave(*lanes):
        chunks = []
        for ln in lanes:
            cl = []
            for op in ln:
                if cl and op[0] == "pe" and cl[-1][-1][0] == "pe":
                    cl[-1].append(op)
                else:
                    cl.append([op])
            chunks.append(cl)
        tot = [max(1, sum(len(c) for c in cl)) for cl in chunks]
        done = [0] * len(lanes)
        pos = [0] * len(lanes)
        while True:
            best = None
            for li, cl in enumerate(chunks):
                if pos[li] < len(cl):
                    fr = done[li] / tot[li]
                    if best is None or fr < best[0]:
                        best = (fr, li)
            if best is None:
                break
            li = best[1]
            for (stream, fn, r, w, dm) in chunks[li][pos[li]]:
                A(stream, fn, r=r, w=w, dma=dm)
            done[li] += len(chunks[li][pos[li]])
            pos[li] += 1
    es = contextlib.ExitStack()
    with es:
        def sb(name, shape, dt=F32):
            return es.enter_context(nc.sbuf_tensor(name, list(shape), dt))

        def psum(name, shape, dt=F32):
            return es.enter_context(nc.psum_tensor(name, list(shape), dt))

        xg = sb("xg", [128, 8, D])
        hT = sb("hT", [128, 8, 1024], BF16)
        actT = sb("actT", [128, 8, 1024], BF16)
        ring = sb("ring", [128, 3, 8, 512], BF16)
        Wd = sb("Wd", [128, 8, 1024], BF16)
        NTS = 15
        TS = sb("TS", [128, NTS, 512])
        SQ = sb("SQ", [128, D])
        E = sb("E", [128, 2, 8, 128])
        gt1bc = sb("gt1bc", [128, D])
        gt2bc = sb("gt2bc", [128, D])
        gfbc = sb("gfbc", [128, D])
        pv = sb("pv", [128, NPV])
        rows = sb("rows", [33, D], BF16)
        bdn = sb("bdn", [32, D])
        wr = sb("wr", [128, 8, NE])
        brbc = sb("brbc", [128, NE])
        cstt = sb("cstt", [128, 640])
        identb = sb("identb", [128, 128], BF16)
        onesf = sb("onesf", [128, 128])
        onesb = sb("onesb", [128, 128], BF16)
        modT = sb("modT", [128, 48])
        AB = sb("AB", [128, 32])
        sm = sb("sm", [128, 64])
        relbt = sb("relbt", [32, 8])
        xn = sb("xn", [128, 2, D], BF16)
        qT = sb("qT", [128, 4, 4, 128], BF16)
        kT = sb("kT", [128, 640], BF16)
        vA = sb("vA", [128, 5, 128], BF16)
        aT = sb("aT", [128, 4, 512], BF16)
        mTh = sb("mTh", [128, 4, 512], BF16)
        hiT = sb("hiT", [128, 4, 128], BF16)
        hiT2 = sb("hiT2", [128, 4, 128], BF16)
        KDT = sb("KDT", [128, 4, 128], BF16)
        SC = sb("SC", [128, 4, 128], BF16)
        BS = sb("BS", [128, 6, 512], BF16)
        state = sb("state", [128, 4, 128])
        statebf = sb("statebf", [128, 4, 128], BF16)
        SBF = sb("SBF", [128, 8, 128], BF16)
        comb = sb("comb", [128, 8, NE])
        CT = sb("CT", [32, 128])
        rt = sb("rt", [128, 160])
        PB = [psum("pb%d" % i, [128, 512]) for i in range(7)]
        PBh = psum("pbh", [128, 1024], BF16)

        def pbn(i):
            return "pb%d" % i

        cond2 = sb("cond2", [128, 8, 2])
        lbd = sm[:, 8:12]
        lb = sm[:, 12:16]
        oml = sm[:, 16:20]
        esink = sm[:, 20:24]
        ss = sm[:, 24:25]
        rs = sm[:, 25:26]
        rstd = sm[:, 26:27]
        ident32 = cstt[:, 0:128]
        XGALL = ["xg%d" % t for t in range(8)]

        def T(i):
            return TS[:, i, :]

        def Tn(i):
            return "T%d" % i

        def dma(eng, out_, in_, r=(), w=(), key=None):
            A(eng, lambda e: e.dma_start(out=out_, in_=in_), r=r, w=w, dma=key)

        dma("sp", pv[:, :], pvec, w=["pv"], key="T:ld0")
        dma("sp", cstt[:, :], cst, w=["cst"], key="T:ld0")
        dma("pool", rows[0:1, :], rowsin[0:1, :], w=["rows0"], key="T:ldp")
        dma("pool", rows[32:33, :], rowsin[1:2, :], w=["rows32"], key="T:ldp")
        dma("sp", bdn[:, :], b_dn, w=["bdn"], key="T:ld0")
        dma("sp", wr[:, :, :], w_router.rearrange("(kc p) e -> p kc e", p=128), w=["wr"], key="T:ld0")
        dma("sp", brbc[:, :], brt.partition_broadcast(128), w=["brbc"], key="T:ld0")
        dma("sp", gfbc[:, :], gfin.partition_broadcast(128), w=["gfbc"], key="T:ld0")
        dma("sp", relbt[:, :], relb, w=["relbt"], key="T:ld0")
        dma("sp", TS[0:32, 0, 0:510], ohb, w=[Tn(0)], key="T:ld0")
        dma("sp", TS[0:8, 1, 0:510], negb, w=[Tn(1)], key="T:ld0")
        A("pool", lambda e: e.memset(onesf[:, :], 1.0), w=["onesf"])
        A("pool", lambda e: e.memset(onesb[:, :], 1.0), w=["onesb"])
        A("pool", lambda e: e.memset(state[:, :, :], 0.0), w=["st%d" % h for h in range(4)])
        A("pool", lambda e: e.memset(statebf[:, :, :], 0.0), w=["sbf%d" % h for h in range(4)])
        A("dve", lambda e: e.tensor_copy(out=identb[:, :], in_=ident32), r=["cst"], w=["identb"])
        for dup in range(2):
            A("act", lambda e, dup=dup: e.activation(out=cond2[:, :, dup], in_=pv[:, PV_C:PV_C + 8], func=AF.Silu), r=["pv"], w=["cond"])
        A("pe", lambda e: e.matmul(PB[5][0:8, 0:510], lhsT=relbt[:, :], rhs=TS[0:32, 0, 0:510], start=True, stop=True),
          r=["relbt", Tn(0)], w=[pbn(5)])
        A("dve", lambda e: e.tensor_tensor(out=TS[0:8, 2, 0:510], in0=PB[5][0:8, 0:510], in1=TS[0:8, 1, 0:510], op=ALU.add),
          r=[pbn(5), Tn(1)], w=[Tn(2)])
        A("act", lambda e: e.activation(out=TS[0:8, 2, 0:510], in_=TS[0:8, 2, 0:510], func=AF.Exp), r=[Tn(2)], w=[Tn(2)])
        for kb in range(2):
            src = TS[0:8, 2, kb * 255:(kb + 1) * 255].unsqueeze(1).to_broadcast([8, 129, 255])
            dma("sp", scr[:, kb, :].rearrange("h (r u) -> h r u", u=255), src, r=[Tn(2)], w=["scr%d" % kb], key="T:scrw")
        for h in range(8):
            for kb in range(2):
                srcap = scr[h, kb, 0:128 * 254].rearrange("(m x) -> m x", x=254)[:, 0:128]
                dma("sp", E[:, kb, h, :], srcap, r=["scr%d" % kb], w=["E%d_%d" % (kb, h)], key="T:scrr")
        ERES = ["E%d_%d" % (kb, h) for kb in range(2) for h in range(8)]
        A("act", lambda e: e.activation(out=esink, in_=pv[:, PV_SINK:PV_SINK + 4], func=AF.Exp), r=["pv"], w=["esink"])
        A("dve", lambda e: e.tensor_tensor(out=lbd, in0=pv[:, PV_LB:PV_LB + 4], in1=pv[:, PV_LB + 4:PV_LB + 8], op=ALU.subtract),
          r=["pv"], w=["lbd"])
        A("act", lambda e: e.activation(out=lb, in_=lbd, func=AF.Sigmoid), r=["lbd"], w=["lb"])
        A("act", lambda e: e.activation(out=oml, in_=lbd, func=AF.Sigmoid, scale=-1.0), r=["lbd"], w=["oml"])
        bu_view = pv[:, PV_BGU:PV_BGU + 512].rearrange("p (q t) -> p q t", t=2)[:, :, 1:2]
        A("dve", lambda e: e.tensor_scalar(bu_view, bu_view, 1.0, None, op0=ALU.add), r=["pv"], w=["pv"])
        for wi in range(6):
            dma("sp", xg[:, :, :], w_ada[:, wi * D:(wi + 1) * D].rearrange("(kc p) n -> p kc n", p=128), w=XGALL, key="ada")
            for j in range(8):
                for kc in range(8):
                    A("pe", lambda e, j=j, kc=kc: e.matmul(PB[6][:, 2 * j:2 * j + 2], lhsT=xg[:, kc, j * 128:(j + 1) * 128],
                                                           rhs=cond2[:, kc, :], start=(kc == 0), stop=(kc == 7)),
                      r=XGALL + ["cond"], w=[pbn(6)])
            A("dve", lambda e, wi=wi: e.tensor_tensor(out=modT[:, wi * 8:(wi + 1) * 8], in0=PB[6][:, 0:16:2],
                                                      in1=pv[:, PV_BADA + wi * 8:PV_BADA + (wi + 1) * 8], op=ALU.add),
              r=[pbn(6), "pv"], w=["modT"])
        for (dst, scw, gcol, shw) in ((0, 1, PV_GMIX, 0), (16, 4, PV_GFFN, 3)):
            A("dve", lambda e, dst=dst, scw=scw: e.tensor_scalar(AB[:, dst:dst + 8], modT[:, scw * 8:(scw + 1) * 8], 1.0, None, op0=ALU.add),
              r=["modT"], w=["AB"])
            A("dve", lambda e, dst=dst, gcol=gcol: e.tensor_tensor(out=AB[:, dst:dst + 8], in0=AB[:, dst:dst + 8], in1=pv[:, gcol:gcol + 8], op=ALU.mult),
              r=["AB", "pv"], w=["AB"])
            A("dve", lambda e, dst=dst, shw=shw: e.tensor_copy(out=AB[:, dst + 8:dst + 16], in_=modT[:, shw * 8:(shw + 1) * 8]),
              r=["modT"], w=["AB"])
        for (wi, dstt, dname) in ((2, gt1bc, "gt1bc"), (5, gt2bc, "gt2bc")):
            for j in range(8):
                dt_ = TS[:, 3 + (j % 2), 0:128]
                A("dve", lambda e, dt_=dt_, wi=wi, j=j: e.tensor_scalar(dt_, ident32, modT[:, wi * 8 + j:wi * 8 + j + 1], None, op0=ALU.mult),
                  r=["cst", "modT"], w=[Tn(3 + (j % 2))])
                pbi = 3 + (j // 4)
                A("pe", lambda e, dt_=dt_, pbi=pbi, j=j: e.matmul(PB[pbi][:, (j % 4) * 128:(j % 4 + 1) * 128], lhsT=onesf[:, :], rhs=dt_,
                                                                   start=True, stop=True),
                  r=["onesf", Tn(3 + (j % 2))], w=[pbn(pbi)])
            for half in range(2):
                A("act", lambda e, dstt=dstt, half=half: e.copy(out=dstt[:, half * 512:(half + 1) * 512], in_=PB[3 + half][:, :]),
                  r=[pbn(3 + half)], w=[dname])

        ring_ctr = [0]

        def rres(k):
            return ["ring%d.%d" % (k, jx) for jx in range(4)]

        def ring_slot():
            k = ring_ctr[0] % 3
            ring_ctr[0] += 1
            return k

        def norm_stats(lt):
            xt = xg[:, lt, :]
            A("act", lambda e: e.activation(out=SQ[:, :], in_=xt, func=AF.Square), r=["xg%d" % lt], w=["SQ"])
            A("dve", lambda e: e.reduce_sum(out=ss, in_=SQ[:, :], axis=AX.X), r=["SQ"], w=["ss"])
            A("act", lambda e: e.activation(out=rs, in_=ss, func=AF.Ln, scale=1.0 / D, bias=EPS), r=["ss"], w=["rs"])
            A("act", lambda e: e.activation(out=rstd, in_=rs, func=AF.Exp, scale=-0.5), r=["rs"], w=["rstd"])

        def norm_T(lt, abase, tile):
            xt = xg[:, lt, :]
            norm_stats(lt)
            xb = xn[:, lt % 2, :]
            xr = "xn%d" % (lt % 2)
            A("act", lambda e: e.activation(out=xb, in_=xt, func=AF.Copy, scale=rstd), r=["xg%d" % lt, "rstd"], w=[xr])
            for kc in range(8):
                A("pe", lambda e, kc=kc: e.transpose(out=PBh[:, kc * 128:(kc + 1) * 128], in_=xb[:, kc * 128:(kc + 1) * 128], identity=identb[:, :]),
                  r=[xr, "identb"], w=["pbh"])
            for kc in range(8):
                A("dve", lambda e, kc=kc: e.tensor_scalar(hT[:, kc, tile * 128:(tile + 1) * 128], PBh[:, kc * 128:(kc + 1) * 128],
                                                          AB[:, abase + kc:abase + kc + 1], AB[:, abase + 8 + kc:abase + 9 + kc],
                                                          op0=ALU.mult, op1=ALU.add),
                  r=["pbh", "AB"], w=["hT%d" % tile])

        HT03 = ["hT%d" % t for t in range(4, 8)]
        fm_ctr = [0]

        def fm_chunk(slot, sres, c0, M=128, pofs=0, pb=None, first=True):
            for kc in range(8):
                A("pe", lambda e, kc=kc: e.matmul(PB[pb][pofs:pofs + M, :], lhsT=ring[:, slot, kc, c0:c0 + M], rhs=hT[:, kc, 512:1024],
                                                  start=(kc == 0), stop=(kc == 7)),
                  r=sres + HT03, w=[pbn(pb)])

        def tok_chunk(slot, sres, c0, rowc0, pb):
            for i in range(4):
                for kc in range(8):
                    A("pe", lambda e, i=i, kc=kc: e.matmul(PB[pb][:, i * 128:(i + 1) * 128], lhsT=hT[:, kc, 512 + i * 128:512 + (i + 1) * 128],
                                                           rhs=ring[:, slot, kc, c0:c0 + 128], start=(kc == 0), stop=False),
                      r=sres + ["hT%d" % (4 + i)], w=[pbn(pb)])
                A("pe", lambda e, i=i: e.matmul(PB[pb][:, i * 128:(i + 1) * 128], lhsT=onesb[32:33, :], rhs=rows[32:33, rowc0:rowc0 + 128],
                                                start=False, stop=True),
                  r=["onesb", "rows32"], w=[pbn(pb)])

        def lowp(fn):
            def g(e):
                with nc.allow_low_precision("sum of 4 masked terms with one non-zero"):
                    return fn(e)
            return g

        def v3(ap, c=4):
            return ap.rearrange("p (c q) -> p c q", c=c)

        def phase1(g, sl):
            s = 2 * g + sl
            lt0 = sl * 4
            for i in range(4):
                lt = lt0 + i
                dma("sp", xg[:, lt, :], x[s * 512 + i * 128:s * 512 + (i + 1) * 128, :], w=["xg%d" % lt], key="x%d" % lt)
            kq = ring_slot()
            dma("pool", ring[:, kq, :, 0:512], w_in[:, 0:512].rearrange("(kc p) n -> p kc n", p=128), w=rres(kq), key="ring%d.0" % kq)
            kkv = ring_slot()
            dma("pool", ring[:, kkv, :, 0:256], w_in[:, 512:768].rearrange("(kc p) n -> p kc n", p=128), w=rres(kkv), key="ring%d.0" % kkv)
            for i in range(4):
                norm_T(lt0 + i, 0, 4 + i)
            for c in range(4):
                pb = c % 2
                fm_chunk(kq, rres(kq), c * 64, M=64, pofs=0, pb=pb)
                fm_chunk(kq, rres(kq), (4 + c) * 64, M=64, pofs=64, pb=pb)
                A("act", lambda e, c=c, pb=pb: e.activation(out=qT[:, :, c, :], in_=v3(PB[pb][:, :]), func=AF.Identity,
                                                            bias=pv[:, PV_BQ + c:PV_BQ + c + 1]),
                  r=[pbn(pb), "pv"], w=["qT"])
            fm_chunk(kkv, rres(kkv), 0, pb=2)
            A("act", lambda e: e.activation(out=kT[:, 128:640], in_=PB[2][:, :], func=AF.Identity, bias=pv[:, PV_BIN + 4:PV_BIN + 5]),
              r=[pbn(2), "pv"], w=["kTm"])
            tok_chunk(kkv, rres(kkv), 128, 0, 4)
            A("dve", lambda e: e.tensor_copy(out=vA[:, 1:5, :], in_=v3(PB[4][:, :])), r=[pbn(4)], w=["vAm"])
            def load_head(h):
                k = ring_slot()
                for jx, base in enumerate((768, 1280, 1792, 2304)):
                    dma("pool", ring[:, k, :, jx * 128:(jx + 1) * 128],
                        w_in[:, base + h * 128:base + (h + 1) * 128].rearrange("(kc p) n -> p kc n", p=128),
                        w=["ring%d.%d" % (k, jx)], key="ring%d.%d" % (k, jx))
                return k
            hslots = [load_head(0)]
            def _attention():
                for i in range(4):
                    jblk = s * 4 + i
                    kbs = [1] if jblk == 0 else [0, 1]
                    for gq in range(2):
                        hp0, hp1 = gq * 64, (gq + 1) * 64
                        for n_, kb in enumerate(kbs):
                            kcol = i * 128 + kb * 128
                            pb = n_
                            A("pe", lambda e, kcol=kcol, pb=pb, i=i, hp0=hp0, hp1=hp1: e.matmul(
                                PB[pb][:, :], lhsT=kT[hp0:hp1, kcol:kcol + 128], rhs=qT[hp0:hp1, i, :, :], start=True, stop=True),
                              r=["kTm", "kTc", "qT"], w=[pbn(pb)])
                            A("act", lambda e, pb=pb, kb=kb: e.activation(out=T(kb), in_=PB[pb][:, :], func=AF.Exp, scale=ATT_SCALE),
                              r=[pbn(pb)], w=[Tn(kb)])
                            A("pool", lambda e, kb=kb, gq=gq: e.tensor_tensor(out=v3(BS[:, kb, :]), in0=v3(T(kb)), in1=E[:, kb, gq * 4:(gq + 1) * 4, :],
                                                                                op=ALU.mult),
                              r=[Tn(kb)] + ERES, w=["PT%d" % kb])
                        for n_, kb in enumerate(kbs):
                            A("pe", lambda e, kb=kb, i=i, st_=(n_ == 0), sp_=(n_ == len(kbs) - 1): e.matmul(
                                PB[2][:, :], lhsT=vA[:, i + kb, :], rhs=BS[:, kb, :], start=st_, stop=sp_),
                              r=["vAm", "vAc", "PT%d" % kb], w=[pbn(2)])
                        for n_, kb in enumerate(kbs):
                            A("pe", lambda e, kb=kb, st_=(n_ == 0), sp_=(n_ == len(kbs) - 1): e.matmul(
                                PB[3][:, :], lhsT=onesb[:, :], rhs=BS[:, kb, :], start=st_, stop=sp_),
                              r=["onesb", "PT%d" % kb], w=[pbn(3)])
                        A("dve", lambda e, hp0=hp0, hp1=hp1: e.tensor_tensor(out=v3(TS[hp0:hp1, 2, :]), in0=v3(PB[3][hp0:hp1, :]),
                                                                              in1=esink[hp0:hp1, :].unsqueeze(2).to_broadcast([64, 4, 128]), op=ALU.add),
                          r=[pbn(3), "esink"], w=[Tn(2)])
                        A("dve", lambda e, hp0=hp0, hp1=hp1: e.reciprocal(out=TS[hp0:hp1, 2, :], in_=TS[hp0:hp1, 2, :]), r=[Tn(2)], w=[Tn(2)])
                        A("dve", lambda e, hp0=hp0, hp1=hp1, i=i: e.tensor_tensor(out=aT[hp0:hp1, :, i * 128:(i + 1) * 128], in0=v3(PB[2][hp0:hp1, :]),
                                                                                   in1=v3(TS[hp0:hp1, 2, :]), op=ALU.mult),
                          r=[pbn(2), Tn(2)], w=["aT"])
                A("pool", lambda e: e.tensor_copy(out=kT[:, 0:128], in_=kT[:, 512:640]), r=["kTm"], w=["kTc"])
                A("pool", lambda e: e.tensor_copy(out=vA[:, 0, :], in_=vA[:, 4, :]), r=["vAm"], w=["vAc"])

            def _hgrn():
                LOGF, BC, DQ, EQ, EK, OSB, OSQ, RSB, XS = 8, 9, 10, 11, 12, 3, 4, 13, 14
                QE, QL, KD, KL = 2, 3, 4, 5
                def inproj(h):
                    p_ = h % 2
                    QF, SG, GS = (5, 0)[p_], (6, 1)[p_], (7, 2)[p_]
                    hv = hiT if p_ == 0 else hiT2
                    hn = "hiT" if p_ == 0 else "hiT2"
                    k = hslots[h]
                    sr = ["ring%d.%d" % (k, jx) for jx in range(4)]
                    fm_chunk(k, [sr[0]], 0, pb=5)
                    A("act", lambda e, h=h: e.activation(out=T(QF), in_=PB[5][:, :], func=AF.Silu, bias=pv[:, PV_BIN + 6 + h:PV_BIN + 7 + h]),
                      r=[pbn(5), "pv"], w=[Tn(QF)])
                    fm_chunk(k, [sr[3]], 384, pb=6)
                    A("act", lambda e, h=h: e.activation(out=T(GS), in_=PB[6][:, :], func=AF.Silu, bias=pv[:, PV_BIN + 18 + h:PV_BIN + 19 + h]),
                      r=[pbn(6), "pv"], w=[Tn(GS)])
                    A("pool", lambda e: e.tensor_scalar(T(GS), T(GS), pv[:, PV_NW:PV_NW + 1], 1.0, op0=ALU.mult, op1=ALU.mult),
                      r=[Tn(GS), "pv"], w=[Tn(GS)])
                    fm_chunk(k, [sr[1]], 128, pb=5)
                    A("act", lambda e, h=h: e.activation(out=T(SG), in_=PB[5][:, :], func=AF.Sigmoid, bias=pv[:, PV_BIN + 10 + h:PV_BIN + 11 + h]),
                      r=[pbn(5), "pv"], w=[Tn(SG)])
                    tok_chunk(k, [sr[2]], 256, 128 + h * 128, 4)
                    A("dve", lambda e: e.tensor_copy(out=hv[:, :, :], in_=v3(PB[4][:, :])), r=[pbn(4)], w=[hn])
                    if h + 1 < 4:
                        hslots.append(load_head(h + 1))

                def gates(h):
                    p_ = h % 2
                    QF, SG, GS = (5, 0)[p_], (6, 1)[p_], (7, 2)[p_]
                    hv = hiT if p_ == 0 else hiT2
                    hn = "hiT" if p_ == 0 else "hiT2"
                    A("dve", lambda e, h=h: e.tensor_scalar(T(SG), T(SG), oml[:, h:h + 1], lb[:, h:h + 1], op0=ALU.mult, op1=ALU.add),
                      r=[Tn(SG), "oml", "lb"], w=[Tn(SG)])
                    A("act", lambda e: e.activation(out=T(LOGF), in_=T(SG), func=AF.Ln), r=[Tn(SG)], w=[Tn(LOGF)])
                    A("dve", lambda e: e.tensor_scalar(T(SG), T(SG), -1.0, 1.0, op0=ALU.mult, op1=ALU.add), r=[Tn(SG)], w=[Tn(SG)])
                    for c in range(8):
                        A("dve", lambda e, c=c: e.tensor_tensor_scan(out=TS[:, BC, c * 64:(c + 1) * 64], data0=onesf[:, 0:64],
                                                                     data1=TS[:, LOGF, c * 64:(c + 1) * 64], initial=0.0, op0=ALU.mult, op1=ALU.add),
                          r=[Tn(LOGF), "onesf"], w=[Tn(BC)])
                    A("act", lambda e: e.activation(out=T(LOGF), in_=T(BC), func=AF.Exp), r=[Tn(BC)], w=[Tn(LOGF)])
                    A("pool", lambda e: e.tensor_tensor(out=BS[:, QE, :], in0=T(QF), in1=T(LOGF), op=ALU.mult), r=[Tn(QF), Tn(LOGF)], w=["QE"])
                    A("dve", lambda e: e.tensor_tensor(out=v3(T(DQ), 8), in0=v3(T(BC), 8),
                                                       in1=v3(T(BC), 8)[:, :, 63:64].to_broadcast([128, 8, 64]), op=ALU.subtract),
                      r=[Tn(BC)], w=[Tn(DQ)])
                    A("act", lambda e: e.activation(out=T(EQ), in_=T(DQ), func=AF.Exp, scale=-1.0), r=[Tn(DQ)], w=[Tn(EQ)])
                    A("pool", lambda e: e.tensor_tensor(out=BS[:, KD, :], in0=T(SG), in1=T(EQ), op=ALU.mult), r=[Tn(SG), Tn(EQ)], w=["KD"])
                    for i in range(4):
                        A("pe", lambda e, i=i: e.transpose(out=PBh[:, i * 128:(i + 1) * 128], in_=BS[:, KD, i * 128:(i + 1) * 128], identity=identb[:, :]),
                          r=["KD", "identb"], w=["pbh"])
                    A("act", lambda e: e.copy(out=KDT[:, :, :], in_=v3(PBh[:, 0:512])), r=["pbh"], w=["KDT"])

                def levels_chain(h):
                    p_ = h % 2
                    QF, SG, GS = (5, 0)[p_], (6, 1)[p_], (7, 2)[p_]
                    hv = hiT if p_ == 0 else hiT2
                    hn = "hiT" if p_ == 0 else "hiT2"
                    LBK = (0, 1, 2, 4)
                    for l, (BL, off) in enumerate(((8, 0), (16, 8), (32, 16), (64, 32))):
                        nb = 512 // BL
                        A("dve", lambda e, BL=BL, off=off, nb=nb: e.tensor_tensor(
                            out=T(DQ).rearrange("p (n j) -> p n j", j=BL), in0=T(BC).rearrange("p (n j) -> p n j", j=BL),
                            in1=T(BC).rearrange("p (n j) -> p n j", j=BL)[:, :, off:off + 1].to_broadcast([128, nb, BL]), op=ALU.subtract),
                          r=[Tn(BC)], w=[Tn(DQ)])
                        if l == 0:
                            A("act", lambda e: e.activation(out=T(EQ), in_=T(DQ), func=AF.Exp), r=[Tn(DQ)], w=[Tn(EQ)])
                            A("act", lambda e: e.activation(out=T(EK), in_=T(DQ), func=AF.Exp, scale=-1.0), r=[Tn(DQ)], w=[Tn(EK)])
                        else:
                            A("dve", lambda e: e.tensor_scalar(T(EQ), T(DQ), 0.0, None, op0=ALU.min), r=[Tn(DQ)], w=[Tn(EQ)])
                            A("dve", lambda e: e.tensor_scalar(T(EK), T(DQ), 0.0, None, op0=ALU.max), r=[Tn(DQ)], w=[Tn(EK)])
                            A("act", lambda e: e.activation(out=T(EQ), in_=T(EQ), func=AF.Exp), r=[Tn(EQ)], w=[Tn(EQ)])
                            A("act", lambda e: e.activation(out=T(EK), in_=T(EK), func=AF.Exp, scale=-1.0), r=[Tn(EK)], w=[Tn(EK)])
                        A("pool", lambda e: e.tensor_tensor(out=BS[:, QL, :], in0=T(QF), in1=T(EQ), op=ALU.mult), r=[Tn(QF), Tn(EQ)], w=["QL"])
                        A("dve", lambda e: e.tensor_tensor(out=BS[:, KL, :], in0=T(SG), in1=T(EK), op=ALU.mult), r=[Tn(SG), Tn(EK)], w=["KL"])
                        for i in range(4):
                            A("pe", lambda e, i=i, l=l: e.matmul(PB[LBK[i]][:, l * 128:(l + 1) * 128], lhsT=BS[:, KL, i * 128:(i + 1) * 128],
                                                                  rhs=BS[:, QL, i * 128:(i + 1) * 128], start=True, stop=True),
                              r=["KL", "QL"], w=[pbn(LBK[i])])
                    for i in range(4):
                        A("dve", lambda e, i=i: e.tensor_tensor(out=T(XS), in0=PB[LBK[i]][:, :], in1=cstt[:, 128:640], op=ALU.mult),
                          r=[pbn(LBK[i]), "cst"], w=[Tn(XS)])
                        A("dve", lowp(lambda e, i=i: e.tensor_reduce(out=SC[:, i, :], in_=T(XS).rearrange("p (l t) -> p t l", l=4), axis=AX.X, op=ALU.add)),
                          r=[Tn(XS)], w=["SC%d" % i])
                    A("act", lambda e, h=h: e.copy(out=SBF[:, 0, :], in_=statebf[:, h, :]), r=["sbf%d" % h], w=["SBF0"])
                    for c in range(8):
                        i, hh = divmod(c, 2)
                        pbk = 3 if hh == 0 else 5
                        A("pe", lambda e, i=i, hh=hh, c=c, pbk=pbk: e.matmul(PB[pbk][:, (c // 2) * 128:(c // 2 + 1) * 128],
                                                                             lhsT=KDT[hh * 64:(hh + 1) * 64, i, :],
                                                                             rhs=hv[hh * 64:(hh + 1) * 64, i, :], start=True, stop=True),
                          r=["KDT", hn], w=[pbn(pbk)])
                    for c in range(8):
                        pbk = 3 if c % 2 == 0 else 5
                        A("dve", lambda e, c=c, h=h, pbk=pbk: e.scalar_tensor_tensor(
                            out=state[:, h, :], in0=state[:, h, :], scalar=TS[:, LOGF, c * 64 + 63:c * 64 + 64],
                            in1=PB[pbk][:, (c // 2) * 128:(c // 2 + 1) * 128], op0=ALU.mult, op1=ALU.add),
                          r=["st%d" % h, Tn(LOGF), pbn(pbk)], w=["st%d" % h])
                        if c < 7:
                            A("act", lambda e, c=c, h=h: e.copy(out=SBF[:, c + 1, :], in_=state[:, h, :]), r=["st%d" % h], w=["SBF%d" % (c + 1)])
                        else:
                            A("act", lambda e, h=h: e.copy(out=statebf[:, h, :], in_=state[:, h, :]), r=["st%d" % h], w=["sbf%d" % h])
                    for i in range(4):
                        A("pe", lambda e, i=i: e.matmul(PB[6][:, i * 128:(i + 1) * 128], lhsT=hv[:, i, :], rhs=SC[:, i, :], start=True, stop=False),
                          r=[hn, "SC%d" % i], w=[pbn(6)])
                        for hh in range(2):
                            c = 2 * i + hh
                            A("pe", lambda e, i=i, hh=hh, c=c: e.matmul(PB[6][:, i * 128 + hh * 64:i * 128 + (hh + 1) * 64], lhsT=SBF[:, c, :],
                                                                        rhs=BS[:, QE, c * 64:(c + 1) * 64], start=False, stop=(hh == 1)),
                              r=["SBF%d" % c, "QE"], w=[pbn(6)])

                def norm(h):
                    p_ = h % 2
                    GS = (7, 2)[p_]
                    A("act", lambda e: e.activation(out=T(OSQ), in_=PB[6][:, :], func=AF.Square), r=[pbn(6)], w=[Tn(OSQ)])
                    A("dve", lambda e: e.tensor_copy(out=T(OSB), in_=PB[6][:, :]), r=[pbn(6)], w=[Tn(OSB)])
                    A("pe", lambda e: e.matmul(PB[5][:, :], lhsT=onesf[:, :], rhs=T(OSQ), start=True, stop=True), r=["onesf", Tn(OSQ)], w=[pbn(5)])
                    A("act", lambda e: e.activation(out=T(RSB), in_=PB[5][:, :], func=AF.Ln, scale=1.0 / 128, bias=EPS), r=[pbn(5)], w=[Tn(RSB)])
                    A("act", lambda e: e.activation(out=T(RSB), in_=T(RSB), func=AF.Exp, scale=-0.5), r=[Tn(RSB)], w=[Tn(RSB)])
                    A("dve", lambda e: e.tensor_tensor(out=T(OSB), in0=T(OSB), in1=T(RSB), op=ALU.mult), r=[Tn(OSB), Tn(RSB)], w=[Tn(OSB)])
                    A("pool", lambda e, h=h: e.tensor_tensor(out=mTh[:, h, :], in0=T(OSB), in1=T(GS), op=ALU.mult), r=[Tn(OSB), Tn(GS)], w=["mTh"])

                def lane_h0():
                    inproj(0)
                    gates(0)
                weave(record(_attention), record(lane_h0))
                for h in range(4):
                    if h == 0:
                        inproj(1)
                    levels_chain(h)
                    if h + 1 < 4:
                        def lane_a(h=h):
                            norm(h)
                            if h + 2 < 4:
                                inproj(h + 2)
                        weave(record(lane_a), record(gates, h + 1))
                    else:
                        norm(h)


            _hgrn()
            if "aT" in dbg_out:
                dma("sp", dbg_out["aT"][s], aT[:, :, :], r=["aT"], key="dbgA%d" % s)
                dma("sp", dbg_out["mTh"][s], mTh[:, :, :], r=["mTh"], key="dbgM%d" % s)
            for i in range(4):
                lt = lt0 + i
                for dh in range(2):
                    pb = dh
                    mms = [(aT[:, c, i * 128:(i + 1) * 128], Wd[:, c, dh * 512:(dh + 1) * 512], ["aT", "Wd0"]) for c in range(4)]
                    mms += [(mTh[:, h, i * 128:(i + 1) * 128], Wd[:, 4 + h, dh * 512:(dh + 1) * 512], ["mTh", "Wd1"]) for h in range(4)]
                    mms += [(onesb[0:1, :], rows[0:1, dh * 512:(dh + 1) * 512], ["onesb", "rows0"])]
                    for n_, (l_, r_, res) in enumerate(mms):
                        A("pe", lambda e, l_=l_, r_=r_, pb=pb, st_=(n_ == 0), sp_=(n_ == len(mms) - 1): e.matmul(PB[pb][:, :], lhsT=l_, rhs=r_, start=st_, stop=sp_),
                          r=res, w=[pbn(pb)])
                    A("dve", lambda e, pb=pb, dh=dh: e.tensor_tensor(out=T(3 + dh), in0=PB[pb][:, :], in1=gt1bc[:, dh * 512:(dh + 1) * 512], op=ALU.mult),
                      r=[pbn(pb), "gt1bc"], w=[Tn(3 + dh)])
                    A("pool", lambda e, lt=lt, dh=dh: e.tensor_tensor(out=xg[:, lt, dh * 512:(dh + 1) * 512], in0=T(3 + dh),
                                                                      in1=xg[:, lt, dh * 512:(dh + 1) * 512], op=ALU.add),
                      r=[Tn(3 + dh), "xg%d" % lt], w=["xg%d" % lt])
                if "x1" in dbg_out:
                    dma("sp", dbg_out["x1"][s * 512 + i * 128:s * 512 + (i + 1) * 128, :], xg[:, lt, :], r=["xg%d" % lt], key="dbgX%d" % lt)

        LG, M8, MSK, NM, EX, SMC = rt[:, 0:32], rt[:, 32:40], rt[:, 40:72], rt[:, 72:73], rt[:, 80:112], rt[:, 73:74]

        def router(g, lt):
            xt = xg[:, lt, :]
            norm_T(lt, 16, lt)
            A("act", lambda e: e.activation(out=SQ[:, :], in_=xt, func=AF.Copy, scale=rstd), r=["xg%d" % lt, "rstd"], w=["SQ"])
            for kc in range(8):
                pbi = 2 + kc // 4
                A("pe", lambda e, kc=kc, pbi=pbi: e.transpose(out=PB[pbi][:, (kc % 4) * 128:(kc % 4 + 1) * 128], in_=SQ[:, kc * 128:(kc + 1) * 128],
                                                              identity=ident32), r=["SQ", "cst"], w=[pbn(pbi)])
            H32 = TS[:, 0:2, :].rearrange("p a (b c) -> p (a b) c", c=128)
            for kc in range(8):
                pbi = 2 + kc // 4
                A("act", lambda e, kc=kc, pbi=pbi: e.activation(out=H32[:, kc, :], in_=PB[pbi][:, (kc % 4) * 128:(kc % 4 + 1) * 128], func=AF.Identity,
                                                                scale=AB[:, 16 + kc:17 + kc], bias=AB[:, 24 + kc:25 + kc]),
                  r=[pbn(pbi), "AB"], w=[Tn(kc // 4)])
            for kc in range(8):
                A("pe", lambda e, kc=kc: e.matmul(PB[4][:, 0:NE], lhsT=H32[:, kc, :], rhs=wr[:, kc, :], start=(kc == 0), stop=(kc == 7)),
                  r=[Tn(kc // 4), "wr"], w=[pbn(4)])
            A("dve", lambda e: e.tensor_tensor(out=LG, in0=PB[4][:, 0:NE], in1=brbc[:, :], op=ALU.add), r=[pbn(4), "brbc"], w=["LG"])
            A("dve", lambda e: e.max(out=M8, in_=LG), r=["LG"], w=["M8"])
            A("dve", lambda e: e.tensor_scalar(MSK, LG, M8[:, 3:4], None, op0=ALU.is_ge), r=["LG", "M8"], w=["MSK"])
            A("dve", lambda e: e.tensor_scalar(NM, M8[:, 0:1], -1.0, None, op0=ALU.mult), r=["M8"], w=["NM"])
            A("act", lambda e: e.activation(out=EX, in_=LG, func=AF.Exp, bias=NM), r=["LG", "NM"], w=["EX"])
            A("dve", lambda e: e.tensor_tensor(out=EX, in0=EX, in1=MSK, op=ALU.mult), r=["EX", "MSK"], w=["EX"])
            A("dve", lambda e: e.reduce_sum(out=SMC, in_=EX, axis=AX.X), r=["EX"], w=["SMC"])
            A("dve", lambda e: e.reciprocal(out=SMC, in_=SMC), r=["SMC"], w=["SMC"])
            A("dve", lambda e, lt=lt: e.tensor_scalar(comb[:, lt, :], EX, SMC, None, op0=ALU.mult), r=["EX", "SMC"], w=["comb%d" % lt])
            if "comb" in dbg_out:
                dma("sp", dbg_out["comb"][g * 1024 + lt * 128:g * 1024 + (lt + 1) * 128, :], comb[:, lt, :], r=["comb%d" % lt], key="dbgC%d" % lt)
            A("pe", lambda e, lt=lt: e.transpose(out=PB[5][0:32, 0:128], in_=comb[:, lt, :], identity=ident32), r=["comb%d" % lt, "cst"], w=[pbn(5)])
            A("act", lambda e: e.copy(out=CT[:, :], in_=PB[5][0:32, 0:128]), r=[pbn(5)], w=["CT"])
            for dh in range(2):
                A("pe", lambda e, dh=dh: e.matmul(PB[dh][:, :], lhsT=CT[:, :], rhs=bdn[:, dh * 512:(dh + 1) * 512], start=True, stop=True),
                  r=["CT", "bdn"], w=[pbn(dh)])
                A("dve", lambda e, dh=dh: e.tensor_tensor(out=T(3 + dh), in0=PB[dh][:, :], in1=gt2bc[:, dh * 512:(dh + 1) * 512], op=ALU.mult),
                  r=[pbn(dh), "gt2bc"], w=[Tn(3 + dh)])
                A("pool", lambda e, lt=lt, dh=dh: e.tensor_tensor(out=xg[:, lt, dh * 512:(dh + 1) * 512], in0=T(3 + dh),
                                                                  in1=xg[:, lt, dh * 512:(dh + 1) * 512], op=ALU.add),
                  r=[Tn(3 + dh), "xg%d" % lt], w=["xg%d" % lt])

        def moe(g):
            pieces = [(e_, pc) for e_ in range(NE) for pc in range(4)]
            slots = {}

            def load_piece(n):
                e_, pc = pieces[n]
                k = ring_slot()
                slots[n] = k
                dma("pool", ring[:, k, :, :], w_gu[e_, :, pc * 512:(pc + 1) * 512].rearrange("(kc p) n -> p kc n", p=128),
                    w=rres(k), key="ring%d.0" % k)

            def load_wd(e_):
                for hf in range(2):
                    dma("pool", Wd[:, hf * 4:(hf + 1) * 4, :], w_dn[e_, hf * 512:(hf + 1) * 512, :].rearrange("(kc p) n -> p kc n", p=128),
                        w=["Wd%d" % hf], key="Wd%d" % hf)

            for n in range(3):
                load_piece(n)
            load_wd(0)
            cnt = 0
            cnt2 = 0
            pend_e = []

            def flush_e():
                while pend_e:
                    U1, T1, ffc, half = pend_e.pop(0)
                    A("dve", lambda e, U1=U1, T1=T1, ffc=ffc, half=half: e.scalar_tensor_tensor(
                        out=actT[:, ffc, half * 512:(half + 1) * 512], in0=T(U1), scalar=8.0, in1=T(T1), op0=ALU.min, op1=ALU.mult),
                      r=[Tn(U1), Tn(T1)], w=["act%d_%d" % (ffc, half)])
            for e_ in range(NE):
                for pc in range(4):
                    n = e_ * 4 + pc
                    k = slots[n]
                    for half in range(2):
                        hres = ["hT%d" % t for t in range(half * 4, half * 4 + 4)]
                        for jj in range(2):
                            ffc = pc * 2 + jj
                            par = cnt % 2
                            cnt += 1
                            pg, pu = par, 2 + par
                            GC, SGM, U1, T1 = 5 + par * 4, 6 + par * 4, 7 + par * 4, 8 + par * 4
                            for kc in range(8):
                                A("pe", lambda e, kc=kc, k=k, jj=jj, pg=pg, half=half: e.matmul(
                                    PB[pg][:, :], lhsT=ring[:, k, kc, jj * 256:jj * 256 + 256:2], rhs=hT[:, kc, half * 512:(half + 1) * 512],
                                    start=(kc == 0), stop=(kc == 7)), r=rres(k) + hres, w=[pbn(pg)])
                            for kc in range(8):
                                A("pe", lambda e, kc=kc, k=k, jj=jj, pu=pu, half=half: e.matmul(
                                    PB[pu][:, :], lhsT=ring[:, k, kc, jj * 256 + 1:jj * 256 + 256:2], rhs=hT[:, kc, half * 512:(half + 1) * 512],
                                    start=(kc == 0), stop=(kc == 7)), r=rres(k) + hres, w=[pbn(pu)])
                            bcol = PV_BGU + e_ * 16 + ffc * 2
                            A("dve", lambda e, pg=pg, GC=GC, bcol=bcol: e.tensor_scalar(T(GC), PB[pg][:, :], pv[:, bcol:bcol + 1], 7.0, op0=ALU.add, op1=ALU.min),
                              r=[pbn(pg), "pv"], w=[Tn(GC)])
                            A("act", lambda e, GC=GC, SGM=SGM: e.activation(out=T(SGM), in_=T(GC), func=AF.Sigmoid, scale=1.702), r=[Tn(GC)], w=[Tn(SGM)])
                            A("dve", lambda e, pu=pu, U1=U1, bcol=bcol: e.tensor_scalar(T(U1), PB[pu][:, :], pv[:, bcol + 1:bcol + 2], -6.0, op0=ALU.add, op1=ALU.max),
                              r=[pbn(pu), "pv"], w=[Tn(U1)])
                            A("pool", lambda e, GC=GC, SGM=SGM, T1=T1: e.tensor_tensor(out=T(T1), in0=T(GC), in1=T(SGM), op=ALU.mult),
                              r=[Tn(GC), Tn(SGM)], w=[Tn(T1)])
                            flush_e()
                            pend_e.append((U1, T1, ffc, half))
                    if n + 3 < len(pieces):
                        load_piece(n + 3)
                for lt in range(8):
                    ares = ["act%d_%d" % (kc, lt // 4) for kc in range(8)]
                    for dh in range(2):
                        py = 4 + cnt2 % 2
                        tm = 3 + cnt2 % 2
                        cnt2 += 1
                        for kc in range(8):
                            A("pe", lambda e, kc=kc, lt=lt, dh=dh, py=py: e.matmul(PB[py][:, :], lhsT=actT[:, kc, lt * 128:(lt + 1) * 128],
                                                                                    rhs=Wd[:, kc, dh * 512:(dh + 1) * 512], start=(kc == 0), stop=(kc == 7)),
                              r=ares + ["Wd0", "Wd1"], w=[pbn(py)])
                        A("dve", lambda e, py=py, tm=tm, dh=dh: e.tensor_tensor(out=T(tm), in0=PB[py][:, :], in1=gt2bc[:, dh * 512:(dh + 1) * 512], op=ALU.mult),
                          r=[pbn(py), "gt2bc"], w=[Tn(tm)])
                        A("dve", lambda e, tm=tm, lt=lt, dh=dh, e_=e_: e.scalar_tensor_tensor(
                            out=xg[:, lt, dh * 512:(dh + 1) * 512], in0=T(tm), scalar=comb[:, lt, e_:e_ + 1], in1=xg[:, lt, dh * 512:(dh + 1) * 512],
                            op0=ALU.mult, op1=ALU.add), r=[Tn(tm), "comb%d" % lt, "xg%d" % lt], w=["xg%d" % lt])
                        if lt == 1 and dh == 1:
                            flush_e()
                    if e_ == NE - 1 and lt >= 2:
                        final(g, [lt - 2])
                if e_ + 1 < NE:
                    load_wd(e_ + 1)

        def final(g, lts=range(8)):
            for lt in lts:
                norm_stats(lt)
                ob = lt % 2
                OUTT = TS[:, 11 + 2 * ob:13 + 2 * ob, :].rearrange("p a b -> p (a b)")
                ores = [Tn(11 + 2 * ob), Tn(12 + 2 * ob)]
                A("dve", lambda e, lt=lt, OUTT=OUTT: e.scalar_tensor_tensor(out=OUTT, in0=xg[:, lt, :], scalar=rstd, in1=gfbc[:, :],
                                                                            op0=ALU.mult, op1=ALU.mult),
                  r=["xg%d" % lt, "rstd", "gfbc"], w=ores)
                dma("sp", out[g * 1024 + lt * 128:g * 1024 + (lt + 1) * 128, :], OUTT, r=ores, key="o%d" % ob)

        for g in range(NGRP):
            for c in range(4):
                for gq in range(2):
                    r0 = (gq * 4 + c) * 64
                    dma("pool", Wd[gq * 64:(gq + 1) * 64, c, :], w_out[r0:r0 + 64, :], w=["Wd0"], key="Wo")
            dma("pool", Wd[:, 4:8, :], w_out[512:1024, :].rearrange("(h p) n -> p h n", p=128), w=["Wd1"], key="Wd1")
            phase1(g, 0)
            phase1(g, 1)
            for lt in range(8):
                router(g, lt)
            moe(g)
            final(g, [6, 7])
        S.emit(nc)
    return nc


def _t5_bucket(dist):
    n = np.maximum(dist, 0)
    nf = np.maximum(n, 1).astype(np.float32)
    large = 16 + (np.log(nf / np.float32(16)) / np.float32(math.log(128 / 16)) * np.float32(16)).astype(np.int32)
    large = np.minimum(large, 31)
    return np.where(n < 16, n, large)


def _static_tables():
    oh = np.zeros((32, 2, 255), np.float32)
    neg = np.zeros((8, 2, 255), np.float32)
    for u in range(255):
        if u <= 127:
            oh[_t5_bucket(np.array(u)), 1, u] = 1.0
            neg[:, 0, u] = NEG
        else:
            oh[_t5_bucket(np.array(u - 127)), 0, u] = 1.0
            neg[:, 1, u] = NEG
    cst = np.zeros((128, 640), np.float32)
    cst[:, 0:128] = np.eye(128, dtype=np.float32)
    s = np.arange(128)[:, None]
    t = np.arange(128)[None, :]
    cst[:, 128:256] = ((s // 8 == t // 8) & (t >= s)).astype(np.float32)
    for li, BL in enumerate((16, 32, 64)):
        cst[:, 256 + li * 128:384 + li * 128] = ((s // BL == t // BL) & (t % BL >= BL // 2) & (s % BL < BL // 2)).astype(np.float32)
    return oh.reshape(32, 510), neg.reshape(8, 510), cst


def _pack_pvec(b, c, g_mix, b_ada, b_in, g_ffn, hg_lb, hg_norm_w, attn_sinks, b_gate_up):
    pvv = np.zeros((128, NPV), np.float32)
    pvv[:, PV_C:PV_C + 8] = c[b].reshape(8, 128).T
    pvv[:, PV_GMIX:PV_GMIX + 8] = g_mix[0].reshape(8, 128).T
    pvv[:, PV_GFFN:PV_GFFN + 8] = g_ffn[0].reshape(8, 128).T
    pvv[:, PV_BADA:PV_BADA + 48] = b_ada[0].reshape(48, 128).T
    pvv[:, PV_BIN:PV_BIN + 22] = b_in[0].reshape(22, 128).T
    bq = b_in[0][0:512].reshape(8, 64)
    for cc in range(4):
        pvv[0:64, PV_BQ + cc] = bq[cc]
        pvv[64:128, PV_BQ + cc] = bq[4 + cc]
    pvv[:, PV_LB:PV_LB + 8] = hg_lb.reshape(2, 4, 128).transpose(2, 0, 1).reshape(128, 8)
    pvv[:, PV_NW] = hg_norm_w[0]
    pvv[0:64, PV_SINK:PV_SINK + 4] = attn_sinks[0][None, 0:4]
    pvv[64:128, PV_SINK:PV_SINK + 4] = attn_sinks[0][None, 4:8]
    pvv[:, PV_BGU:PV_BGU + 512] = b_gate_up[0].reshape(32, 8, 128, 2).transpose(2, 0, 1, 3).reshape(128, 512)
    return pvv


_NC_CACHE = {}


def run(inputs, SEQ=4096, cores=8, dbg=()):
    f = lambda a: np.ascontiguousarray(np.asarray(a, dtype=np.float32))
    x = f(inputs["x"])
    oh, neg, cst = _static_tables()
    b_in = f(inputs["b_in"])
    rowsin = np.zeros((2, D), np.float32)
    rowsin[0] = f(inputs["b_out"])[0]
    rowsin[1, 0:128] = b_in[0][640:768]
    rowsin[1, 128:640] = b_in[0][1792:2304]
    shared = dict(
        w_ada=f(inputs["w_ada"])[0], w_in=f(inputs["w_in"])[0], rowsin=rowsin, relb=f(inputs["rel_bias"]), ohb=oh, negb=neg, cst=cst,
        w_out=f(inputs["w_out"])[0], w_router=f(inputs["w_router"])[0], brt=f(inputs["b_router"]).reshape(1, NE),
        w_gu=f(inputs["w_gate_up"])[0], w_dn=f(inputs["w_down"])[0], b_dn=f(inputs["b_down"])[0], gfin=f(inputs["g_final"]).reshape(1, D))
    in_maps = []
    for b in range(cores):
        m = dict(shared)
        m["x"] = np.ascontiguousarray(x[b, :SEQ])
        m["pvec"] = _pack_pvec(b, f(inputs["c"]), f(inputs["g_mix"]), f(inputs["b_ada"]), b_in, f(inputs["g_ffn"]), f(inputs["hg_lb"]),
                               f(inputs["hg_norm_w"]), f(inputs["attn_sinks"]), f(inputs["b_gate_up"]))
        in_maps.append(m)
    key = (SEQ, tuple(dbg))
    if key not in _NC_CACHE:
        _NC_CACHE[key] = build(SEQ, dbg)
    res = run_bass_kernel_spmd(_NC_CACHE[key], in_maps, core_ids=list(range(cores)))
    return res


def kernel(**inputs):
    res = run(inputs, SEQ=4096, cores=8)
    return np.stack([np.asarray(r["out"], dtype=np.float32) for r in res.results], axis=0)
```

```python
import contextlib
import math

import numpy as np
import concourse.bass as bass
import concourse.mybir as mybir
from concourse.bass_utils import run_bass_kernel_spmd

F32 = mybir.dt.float32
BF16 = mybir.dt.bfloat16
AF = mybir.ActivationFunctionType
ALU = mybir.AluOpType
AX = mybir.AxisListType

D = 1024
NE = 32
EPS = 1e-5
ATT_SCALE = 64 ** -0.5
IN_COLS = 2816
NEG = -30000.0

PV_C, PV_GMIX, PV_GFFN, PV_BADA, PV_BIN, PV_BQ, PV_LB, PV_NW, PV_SINK, PV_BGU = 0, 8, 16, 24, 72, 94, 98, 106, 107, 111
NPV = PV_BGU + 512


class _Op:
    __slots__ = ("stream", "fn", "dma", "deps", "sig", "tok")


class Sched:
    STREAMS = ("pe", "act", "dve", "pool", "sp")

    def __init__(self):
        self.ops = {s: [] for s in self.STREAMS}
        self.last_write = {}
        self.readers = {}
        self.dma_counts = {}

    def add(self, stream, fn, r=(), w=(), dma=None):
        op = _Op()
        op.stream, op.fn, op.dma, op.sig, op.tok = stream, fn, dma is not None, False, None
        pr = [x for x in r if x.startswith("pb")]
        if pr:
            r = [x for x in r if not x.startswith("pb")]
            w = list(w) + pr
        deps = {}
        for x in r:
            d = self.last_write.get(x)
            if d is not None:
                deps[id(d)] = (d, True)
        for x in w:
            d = self.last_write.get(x)
            if d is not None and id(d) not in deps:
                deps[id(d)] = (d, False)
            for rd in self.readers.get(x, ()):
                if id(rd) not in deps:
                    deps[id(rd)] = (rd, False)
        op.deps = []
        for d, raw in deps.values():
            if d.dma or d.stream != stream or op.dma:
                need = True
            else:
                need = stream != "pe"
            if need:
                op.deps.append(d)
                d.sig = True
        for x in r:
            self.readers.setdefault(x, []).append(op)
        for x in w:
            self.last_write[x] = op
            self.readers[x] = []
        if op.dma:
            c = self.dma_counts.get(dma, 0) + 16
            self.dma_counts[dma] = c
            op.tok = (("dma", dma), c)
        self.ops[stream].append(op)
        return op

    def emit(self, nc):
        for s in self.STREAMS:
            c = 0
            for op in self.ops[s]:
                if not op.dma and op.sig:
                    c += 1
                    op.tok = (("eng", s), c)
        keys = [("eng", s) for s in self.STREAMS] + [("dma", k) for k in self.dma_counts]
        with contextlib.ExitStack() as es:
            sems = {}
            for k in keys:
                sems[k] = es.enter_context(nc.semaphore("s_%s_%s" % (k[0], k[1])))
            block = es.enter_context(nc.Block())
            sched = self

            def run_stream(s, eng):
                waited = {}
                for op in sched.ops[s]:
                    need = {}
                    for d in op.deps:
                        k, v = d.tok
                        if k[0] == "dma" and k[1].startswith("T:"):
                            v = sched.dma_counts[k[1]]
                        if need.get(k, 0) < v:
                            need[k] = v
                    for k, v in need.items():
                        if waited.get(k, 0) < v:
                            eng.wait_ge(sems[k], v)
                            waited[k] = v
                    ins = op.fn(eng)
                    if op.dma:
                        ins.then_inc(sems[op.tok[0]], 16)
                    elif op.sig:
                        ins.then_inc(sems[op.tok[0]], 1)
                if s == "sp":
                    for k, c in sched.dma_counts.items():
                        if waited.get(("dma", k), 0) < c:
                            eng.wait_ge(sems[("dma", k)], c)

            @block.tensor
            def _(e):
                run_stream("pe", e)

            @block.scalar
            def _(e):
                run_stream("act", e)

            @block.vector
            def _(e):
                run_stream("dve", e)

            @block.gpsimd
            def _(e):
                run_stream("pool", e)

            @block.sync
            def _(e):
                run_stream("sp", e)


def build(SEQ=4096, dbg=()):
    nc = bass.Bass("TRN2", target_bir_lowering=False)
    NGRP = SEQ // 1024

    def din(name, shape):
        return nc.dram_tensor(name, list(shape), F32, kind="ExternalInput").ap()

    x = din("x", [SEQ, D])
    pvec = din("pvec", [128, NPV])
    w_ada = din("w_ada", [D, 6 * D])
    w_in = din("w_in", [D, IN_COLS])
    rowsin = din("rowsin", [2, D])
    relb = din("relb", [32, 8])
    ohb = din("ohb", [32, 510])
    negb = din("negb", [8, 510])
    cst = din("cst", [128, 640])
    w_out = din("w_out", [D, D])
    w_router = din("w_router", [D, NE])
    brt = din("brt", [1, NE])
    w_gu = din("w_gu", [NE, D, 2 * D])
    w_dn = din("w_dn", [NE, D, D])
    b_dn = din("b_dn", [NE, D])
    gfin = din("gfin", [1, D])
    out = nc.dram_tensor("out", [SEQ, D], F32, kind="ExternalOutput").ap()
    scr = nc.dram_tensor("scr", [8, 2, 129 * 255], F32, kind="Internal").ap()
    dbg_out = {}
    if "x1" in dbg:
        dbg_out["x1"] = nc.dram_tensor("dbg_x1", [SEQ, D], F32, kind="ExternalOutput").ap()
    if "comb" in dbg:
        dbg_out["comb"] = nc.dram_tensor("dbg_comb", [SEQ, NE], F32, kind="ExternalOutput").ap()
    if "mix" in dbg:
        dbg_out["aT"] = nc.dram_tensor("dbg_aT", [SEQ // 512, 128, 4, 512], BF16, kind="ExternalOutput").ap()
        dbg_out["mTh"] = nc.dram_tensor("dbg_mTh", [SEQ // 512, 128, 4, 512], BF16, kind="ExternalOutput").ap()

    S = Sched()
    lane_stack = []

    def A(stream, fn, r=(), w=(), dma=None):
        if lane_stack:
            lane_stack[-1].append((stream, fn, tuple(r), tuple(w), dma))
        else:
            S.add(stream, fn, r=r, w=w, dma=dma)

    def record(f, *args):
        lane_stack.append([])
        f(*args)
        return lane_stack.pop()

    def weave(*lanes):
        chunks = []
        for ln in lanes:
            cl = []
            for op in ln:
                if cl and op[0] == "pe" and cl[-1][-1][0] == "pe":
                    cl[-1].append(op)
                else:
                    cl.append([op])
            chunks.append(cl)
        tot = [max(1, sum(len(c) for c in cl)) for cl in chunks]
        done = [0] * len(lanes)
        pos = [0] * len(lanes)
        while True:
            best = None
            for li, cl in enumerate(chunks):
                if pos[li] < len(cl):
                    fr = done[li] / tot[li]
                    if best is None or fr < best[0]:
                        best = (fr, li)
            if best is None:
                break
            li = best[1]
            for (stream, fn, r, w, dm) in chunks[li][pos[li]]:
                A(stream, fn, r=r, w=w, dma=dm)
            done[li] += len(chunks[li][pos[li]])
            pos[li] += 1
    es = contextlib.ExitStack()
    with es:
        def sb(name, shape, dt=F32):
            return es.enter_context(nc.sbuf_tensor(name, list(shape), dt))

        def psum(name, shape, dt=F32):
            return es.enter_context(nc.psum_tensor(name, list(shape), dt))

        xg = sb("xg", [128, 8, D])
        hT = sb("hT", [128, 8, 1024], BF16)
        actT = sb("actT", [128, 8, 1024], BF16)
        ring = sb("ring", [128, 3, 8, 512], BF16)
        Wd = sb("Wd", [128, 8, 1024], BF16)
        NTS = 15
        TS = sb("TS", [128, NTS, 512])
        SQ = sb("SQ", [128, D])
        E = sb("E", [128, 2, 8, 128])
        gt1bc = sb("gt1bc", [128, D])
        gt2bc = sb("gt2bc", [128, D])
        gfbc = sb("gfbc", [128, D])
        pv = sb("pv", [128, NPV])
        rows = sb("rows", [33, D], BF16)
        bdn = sb("bdn", [32, D])
        wr = sb("wr", [128, 8, NE])
        brbc = sb("brbc", [128, NE])
        cstt = sb("cstt", [128, 640])
        identb = sb("identb", [128, 128], BF16)
        onesf = sb("onesf", [128, 128])
        onesb = sb("onesb", [128, 128], BF16)
        modT = sb("modT", [128, 48])
        AB = sb("AB", [128, 32])
        sm = sb("sm", [128, 64])
        relbt = sb("relbt", [32, 8])
        xn = sb("xn", [128, 2, D], BF16)
        qT = sb("qT", [128, 4, 4, 128], BF16)
        kT = sb("kT", [128, 640], BF16)
        vA = sb("vA", [128, 5, 128], BF16)
        aT = sb("aT", [128, 4, 512], BF16)
        mTh = sb("mTh", [128, 4, 512], BF16)
        hiT = sb("hiT", [128, 4, 128], BF16)
        hiT2 = sb("hiT2", [128, 4, 128], BF16)
        KDT = sb("KDT", [128, 4, 128], BF16)
        SC = sb("SC", [128, 4, 128], BF16)
        BS = sb("BS", [128, 6, 512], BF16)
        state = sb("state", [128, 4, 128])
        statebf = sb("statebf", [128, 4, 128], BF16)
        SBF = sb("SBF", [128, 8, 128], BF16)
        comb = sb("comb", [128, 8, NE])
        CT = sb("CT", [32, 128])
        rt = sb("rt", [128, 160])
        PB = [psum("pb%d" % i, [128, 512]) for i in range(7)]
        PBh = psum("pbh", [128, 1024], BF16)

        def pbn(i):
            return "pb%d" % i

        cond2 = sb("cond2", [128, 8, 2])
        lbd = sm[:, 8:12]
        lb = sm[:, 12:16]
        oml = sm[:, 16:20]
        esink = sm[:, 20:24]
        ss = sm[:, 24:25]
        rs = sm[:, 25:26]
        rstd = sm[:, 26:27]
        ident32 = cstt[:, 0:128]
        XGALL = ["xg%d" % t for t in range(8)]

        def T(i):
            return TS[:, i, :]

        def Tn(i):
            return "T%d" % i

        def dma(eng, out_, in_, r=(), w=(), key=None):
            A(eng, lambda e: e.dma_start(out=out_, in_=in_), r=r, w=w, dma=key)

        dma("sp", pv[:, :], pvec, w=["pv"], key="T:ld0")
        dma("sp", cstt[:, :], cst, w=["cst"], key="T:ld0")
        dma("pool", rows[0:1, :], rowsin[0:1, :], w=["rows0"], key="T:ldp")
        dma("pool", rows[32:33, :], rowsin[1:2, :], w=["rows32"], key="T:ldp")
        dma("sp", bdn[:, :], b_dn, w=["bdn"], key="T:ld0")
        dma("sp", wr[:, :, :], w_router.rearrange("(kc p) e -> p kc e", p=128), w=["wr"], key="T:ld0")
        dma("sp", brbc[:, :], brt.partition_broadcast(128), w=["brbc"], key="T:ld0")
        dma("sp", gfbc[:, :], gfin.partition_broadcast(128), w=["gfbc"], key="T:ld0")
        dma("sp", relbt[:, :], relb, w=["relbt"], key="T:ld0")
        dma("sp", TS[0:32, 0, 0:510], ohb, w=[Tn(0)], key="T:ld0")
        dma("sp", TS[0:8, 1, 0:510], negb, w=[Tn(1)], key="T:ld0")
        A("pool", lambda e: e.memset(onesf[:, :], 1.0), w=["onesf"])
        A("pool", lambda e: e.memset(onesb[:, :], 1.0), w=["onesb"])
        A("pool", lambda e: e.memset(state[:, :, :], 0.0), w=["st%d" % h for h in range(4)])
        A("pool", lambda e: e.memset(statebf[:, :, :], 0.0), w=["sbf%d" % h for h in range(4)])
        A("dve", lambda e: e.tensor_copy(out=identb[:, :], in_=ident32), r=["cst"], w=["identb"])
        for dup in range(2):
            A("act", lambda e, dup=dup: e.activation(out=cond2[:, :, dup], in_=pv[:, PV_C:PV_C + 8], func=AF.Silu), r=["pv"], w=["cond"])
        A("pe", lambda e: e.matmul(PB[5][0:8, 0:510], lhsT=relbt[:, :], rhs=TS[0:32, 0, 0:510], start=True, stop=True),
          r=["relbt", Tn(0)], w=[pbn(5)])
        A("dve", lambda e: e.tensor_tensor(out=TS[0:8, 2, 0:510], in0=PB[5][0:8, 0:510], in1=TS[0:8, 1, 0:510], op=ALU.add),
          r=[pbn(5), Tn(1)], w=[Tn(2)])
        A("act", lambda e: e.activation(out=TS[0:8, 2, 0:510], in_=TS[0:8, 2, 0:510], func=AF.Exp), r=[Tn(2)], w=[Tn(2)])
        for kb in range(2):
            src = TS[0:8, 2, kb * 255:(kb + 1) * 255].unsqueeze(1).to_broadcast([8, 129, 255])
            dma("sp", scr[:, kb, :].rearrange("h (r u) -> h r u", u=255), src, r=[Tn(2)], w=["scr%d" % kb], key="T:scrw")
        for h in range(8):
            for kb in range(2):
                srcap = scr[h, kb, 0:128 * 254].rearrange("(m x) -> m x", x=254)[:, 0:128]
                dma("sp", E[:, kb, h, :], srcap, r=["scr%d" % kb], w=["E%d_%d" % (kb, h)], key="T:scrr")
        ERES = ["E%d_%d" % (kb, h) for kb in range(2) for h in range(8)]
        A("act", lambda e: e.activation(out=esink, in_=pv[:, PV_SINK:PV_SINK + 4], func=AF.Exp), r=["pv"], w=["esink"])
        A("dve", lambda e: e.tensor_tensor(out=lbd, in0=pv[:, PV_LB:PV_LB + 4], in1=pv[:, PV_LB + 4:PV_LB + 8], op=ALU.subtract),
          r=["pv"], w=["lbd"])
        A("act", lambda e: e.activation(out=lb, in_=lbd, func=AF.Sigmoid), r=["lbd"], w=["lb"])
        A("act", lambda e: e.activation(out=oml, in_=lbd, func=AF.Sigmoid, scale=-1.0), r=["lbd"], w=["oml"])
        bu_view = pv[:, PV_BGU:PV_BGU + 512].rearrange("p (q t) -> p q t", t=2)[:, :, 1:2]
        A("dve", lambda e: e.tensor_scalar(bu_view, bu_view, 1.0, None, op0=ALU.add), r=["pv"], w=["pv"])
        for wi in range(6):
            dma("sp", xg[:, :, :], w_ada[:, wi * D:(wi + 1) * D].rearrange("(kc p) n -> p kc n", p=128), w=XGALL, key="ada")
            for j in range(8):
                for kc in range(8):
                    A("pe", lambda e, j=j, kc=kc: e.matmul(PB[6][:, 2 * j:2 * j + 2], lhsT=xg[:, kc, j * 128:(j + 1) * 128],
                                                           rhs=cond2[:, kc, :], start=(kc == 0), stop=(kc == 7)),
                      r=XGALL + ["cond"], w=[pbn(6)])
            A("dve", lambda e, wi=wi: e.tensor_tensor(out=modT[:, wi * 8:(wi + 1) * 8], in0=PB[6][:, 0:16:2],
                                                      in1=pv[:, PV_BADA + wi * 8:PV_BADA + (wi + 1) * 8], op=ALU.add),
              r=[pbn(6), "pv"], w=["modT"])
        for (dst, scw, gcol, shw) in ((0, 1, PV_GMIX, 0), (16, 4, PV_GFFN, 3)):
            A("dve", lambda e, dst=dst, scw=scw: e.tensor_scalar(AB[:, dst:dst + 8], modT[:, scw * 8:(scw + 1) * 8], 1.0, None, op0=ALU.add),
              r=["modT"], w=["AB"])
            A("dve", lambda e, dst=dst, gcol=gcol: e.tensor_tensor(out=AB[:, dst:dst + 8], in0=AB[:, dst:dst + 8], in1=pv[:, gcol:gcol + 8], op=ALU.mult),
              r=["AB", "pv"], w=["AB"])
            A("dve", lambda e, dst=dst, shw=shw: e.tensor_copy(out=AB[:, dst + 8:dst + 16], in_=modT[:, shw * 8:(shw + 1) * 8]),
              r=["modT"], w=["AB"])
        for (wi, dstt, dname) in ((2, gt1bc, "gt1bc"), (5, gt2bc, "gt2bc")):
            for j in range(8):
                dt_ = TS[:, 3 + (j % 2), 0:128]
                A("dve", lambda e, dt_=dt_, wi=wi, j=j: e.tensor_scalar(dt_, ident32, modT[:, wi * 8 + j:wi * 8 + j + 1], None, op0=ALU.mult),
                  r=["cst", "modT"], w=[Tn(3 + (j % 2))])
                pbi = 3 + (j // 4)
                A("pe", lambda e, dt_=dt_, pbi=pbi, j=j: e.matmul(PB[pbi][:, (j % 4) * 128:(j % 4 + 1) * 128], lhsT=onesf[:, :], rhs=dt_,
                                                                   start=True, stop=True),
                  r=["onesf", Tn(3 + (j % 2))], w=[pbn(pbi)])
            for half in range(2):
                A("act", lambda e, dstt=dstt, half=half: e.copy(out=dstt[:, half * 512:(half + 1) * 512], in_=PB[3 + half][:, :]),
                  r=[pbn(3 + half)], w=[dname])

        ring_ctr = [0]

        def rres(k):
            return ["ring%d.%d" % (k, jx) for jx in range(4)]

        def ring_slot():
            k = ring_ctr[0] % 3
            ring_ctr[0] += 1
            return k

        def norm_stats(lt):
            xt = xg[:, lt, :]
            A("act", lambda e: e.activation(out=SQ[:, :], in_=xt, func=AF.Square), r=["xg%d" % lt], w=["SQ"])
            A("dve", lambda e: e.reduce_sum(out=ss, in_=SQ[:, :], axis=AX.X), r=["SQ"], w=["ss"])
            A("act", lambda e: e.activation(out=rs, in_=ss, func=AF.Ln, scale=1.0 / D, bias=EPS), r=["ss"], w=["rs"])
            A("act", lambda e: e.activation(out=rstd, in_=rs, func=AF.Exp, scale=-0.5), r=["rs"], w=["rstd"])

        def norm_T(lt, abase, tile):
            xt = xg[:, lt, :]
            norm_stats(lt)
            xb = xn[:, lt % 2, :]
            xr = "xn%d" % (lt % 2)
            A("act", lambda e: e.activation(out=xb, in_=xt, func=AF.Copy, scale=rstd), r=["xg%d" % lt, "rstd"], w=[xr])
            for kc in range(8):
                A("pe", lambda e, kc=kc: e.transpose(out=PBh[:, kc * 128:(kc + 1) * 128], in_=xb[:, kc * 128:(kc + 1) * 128], identity=identb[:, :]),
                  r=[xr, "identb"], w=["pbh"])
            for kc in range(8):
                A("dve", lambda e, kc=kc: e.tensor_scalar(hT[:, kc, tile * 128:(tile + 1) * 128], PBh[:, kc * 128:(kc + 1) * 128],
                                                          AB[:, abase + kc:abase + kc + 1], AB[:, abase + 8 + kc:abase + 9 + kc],
                                                          op0=ALU.mult, op1=ALU.add),
                  r=["pbh", "AB"], w=["hT%d" % tile])

        HT03 = ["hT%d" % t for t in range(4, 8)]
        fm_ctr = [0]

        def fm_chunk(slot, sres, c0, M=128, pofs=0, pb=None, first=True):
            for kc in range(8):
                A("pe", lambda e, kc=kc: e.matmul(PB[pb][pofs:pofs + M, :], lhsT=ring[:, slot, kc, c0:c0 + M], rhs=hT[:, kc, 512:1024],
                                                  start=(kc == 0), stop=(kc == 7)),
                  r=sres + HT03, w=[pbn(pb)])

        def tok_chunk(slot, sres, c0, rowc0, pb):
            for i in range(4):
                for kc in range(8):
                    A("pe", lambda e, i=i, kc=kc: e.matmul(PB[pb][:, i * 128:(i + 1) * 128], lhsT=hT[:, kc, 512 + i * 128:512 + (i + 1) * 128],
                                                           rhs=ring[:, slot, kc, c0:c0 + 128], start=(kc == 0), stop=False),
                      r=sres + ["hT%d" % (4 + i)], w=[pbn(pb)])
                A("pe", lambda e, i=i: e.matmul(PB[pb][:, i * 128:(i + 1) * 128], lhsT=onesb[32:33, :], rhs=rows[32:33, rowc0:rowc0 + 128],
                                                start=False, stop=True),
                  r=["onesb", "rows32"], w=[pbn(pb)])

        def lowp(fn):
            def g(e):
                with nc.allow_low_precision("sum of 4 masked terms with one non-zero"):
                    return fn(e)
            return g

        def v3(ap, c=4):
            return ap.rearrange("p (c q) -> p c q", c=c)

        def phase1(g, sl):
            s = 2 * g + sl
            lt0 = sl * 4
            for i in range(4):
                lt = lt0 + i
                dma("sp", xg[:, lt, :], x[s * 512 + i * 128:s * 512 + (i + 1) * 128, :], w=["xg%d" % lt], key="x%d" % lt)
            kq = ring_slot()
            dma("pool", ring[:, kq, :, 0:512], w_in[:, 0:512].rearrange("(kc p) n -> p kc n", p=128), w=rres(kq), key="ring%d.0" % kq)
            kkv = ring_slot()
            dma("pool", ring[:, kkv, :, 0:256], w_in[:, 512:768].rearrange("(kc p) n -> p kc n", p=128), w=rres(kkv), key="ring%d.0" % kkv)
            for i in range(4):
                norm_T(lt0 + i, 0, 4 + i)
            for c in range(4):
                pb = c % 2
                fm_chunk(kq, rres(kq), c * 64, M=64, pofs=0, pb=pb)
                fm_chunk(kq, rres(kq), (4 + c) * 64, M=64, pofs=64, pb=pb)
                A("act", lambda e, c=c, pb=pb: e.activation(out=qT[:, :, c, :], in_=v3(PB[pb][:, :]), func=AF.Identity,
                                                            bias=pv[:, PV_BQ + c:PV_BQ + c + 1]),
                  r=[pbn(pb), "pv"], w=["qT"])
            fm_chunk(kkv, rres(kkv), 0, pb=2)
            A("act", lambda e: e.activation(out=kT[:, 128:640], in_=PB[2][:, :], func=AF.Identity, bias=pv[:, PV_BIN + 4:PV_BIN + 5]),
              r=[pbn(2), "pv"], w=["kTm"])
            tok_chunk(kkv, rres(kkv), 128, 0, 4)
            A("dve", lambda e: e.tensor_copy(out=vA[:, 1:5, :], in_=v3(PB[4][:, :])), r=[pbn(4)], w=["vAm"])
            def load_head(h):
                k = ring_slot()
                for jx, base in enumerate((768, 1280, 1792, 2304)):
                    dma("pool", ring[:, k, :, jx * 128:(jx + 1) * 128],
                        w_in[:, base + h * 128:base + (h + 1) * 128].rearrange("(kc p) n -> p kc n", p=128),
                        w=["ring%d.%d" % (k, jx)], key="ring%d.%d" % (k, jx))
                return k
            hslots = [load_head(0)]
            def _attention():
                for i in range(4):
                    jblk = s * 4 + i
                    kbs = [1] if jblk == 0 else [0, 1]
                    for gq in range(2):
                        hp0, hp1 = gq * 64, (gq + 1) * 64
                        for n_, kb in enumerate(kbs):
                            kcol = i * 128 + kb * 128
                            pb = n_
                            A("pe", lambda e, kcol=kcol, pb=pb, i=i, hp0=hp0, hp1=hp1: e.matmul(
                                PB[pb][:, :], lhsT=kT[hp0:hp1, kcol:kcol + 128], rhs=qT[hp0:hp1, i, :, :], start=True, stop=True),
                              r=["kTm", "kTc", "qT"], w=[pbn(pb)])
                            A("act", lambda e, pb=pb, kb=kb: e.activation(out=T(kb), in_=PB[pb][:, :], func=AF.Exp, scale=ATT_SCALE),
                              r=[pbn(pb)], w=[Tn(kb)])
                            A("pool", lambda e, kb=kb, gq=gq: e.tensor_tensor(out=v3(BS[:, kb, :]), in0=v3(T(kb)), in1=E[:, kb, gq * 4:(gq + 1) * 4, :],
                                                                                op=ALU.mult),
                              r=[Tn(kb)] + ERES, w=["PT%d" % kb])
                        for n_, kb in enumerate(kbs):
                            A("pe", lambda e, kb=kb, i=i, st_=(n_ == 0), sp_=(n_ == len(kbs) - 1): e.matmul(
                                PB[2][:, :], lhsT=vA[:, i + kb, :], rhs=BS[:, kb, :], start=st_, stop=sp_),
                              r=["vAm", "vAc", "PT%d" % kb], w=[pbn(2)])
                        for n_, kb in enumerate(kbs):
                            A("pe", lambda e, kb=kb, st_=(n_ == 0), sp_=(n_ == len(kbs) - 1): e.matmul(
                                PB[3][:, :], lhsT=onesb[:, :], rhs=BS[:, kb, :], start=st_, stop=sp_),
                              r=["onesb", "PT%d" % kb], w=[pbn(3)])
                        A("dve", lambda e, hp0=hp0, hp1=hp1: e.tensor_tensor(out=v3(TS[hp0:hp1, 2, :]), in0=v3(PB[3][hp0:hp1, :]),
                                                                              in1=esink[hp0:hp1, :].unsqueeze(2).to_broadcast([64, 4, 128]), op=ALU.add),
                          r=[pbn(3), "esink"], w=[Tn(2)])
                        A("dve", lambda e, hp0=hp0, hp1=hp1: e.reciprocal(out=TS[hp0:hp1, 2, :], in_=TS[hp0:hp1, 2, :]), r=[Tn(2)], w=[Tn(2)])
                        A("dve", lambda e, hp0=hp0, hp1=hp1, i=i: e.tensor_tensor(out=aT[hp0:hp1, :, i * 128:(i + 1) * 128], in0=v3(PB[2][hp0:hp1, :]),
                                                                                   in1=v3(TS[hp0:hp1, 2, :]), op=ALU.mult),
                          r=[pbn(2), Tn(2)], w=["aT"])
                A("pool", lambda e: e.tensor_copy(out=kT[:, 0:128], in_=kT[:, 512:640]), r=["kTm"], w=["kTc"])
                A("pool", lambda e: e.tensor_copy(out=vA[:, 0, :], in_=vA[:, 4, :]), r=["vAm"], w=["vAc"])

            def _hgrn():
                LOGF, BC, DQ, EQ, EK, OSB, OSQ, RSB, XS = 8, 9, 10, 11, 12, 3, 4, 13, 14
                QE, QL, KD, KL = 2, 3, 4, 5
                def inproj(h):
                    p_ = h % 2
                    QF, SG, GS = (5, 0)[p_], (6, 1)[p_], (7, 2)[p_]
                    hv = hiT if p_ == 0 else hiT2
                    hn = "hiT" if p_ == 0 else "hiT2"
                    k = hslots[h]
                    sr = ["ring%d.%d" % (k, jx) for jx in range(4)]
                    fm_chunk(k, [sr[0]], 0, pb=5)
                    A("act", lambda e, h=h: e.activation(out=T(QF), in_=PB[5][:, :], func=AF.Silu, bias=pv[:, PV_BIN + 6 + h:PV_BIN + 7 + h]),
                      r=[pbn(5), "pv"], w=[Tn(QF)])
                    fm_chunk(k, [sr[3]], 384, pb=6)
                    A("act", lambda e, h=h: e.activation(out=T(GS), in_=PB[6][:, :], func=AF.Silu, bias=pv[:, PV_BIN + 18 + h:PV_BIN + 19 + h]),
                      r=[pbn(6), "pv"], w=[Tn(GS)])
                    A("pool", lambda e: e.tensor_scalar(T(GS), T(GS), pv[:, PV_NW:PV_NW + 1], 1.0, op0=ALU.mult, op1=ALU.mult),
                      r=[Tn(GS), "pv"], w=[Tn(GS)])
                    fm_chunk(k, [sr[1]], 128, pb=5)
                    A("act", lambda e, h=h: e.activation(out=T(SG), in_=PB[5][:, :], func=AF.Sigmoid, bias=pv[:, PV_BIN + 10 + h:PV_BIN + 11 + h]),
                      r=[pbn(5), "pv"], w=[Tn(SG)])
                    tok_chunk(k, [sr[2]], 256, 128 + h * 128, 4)
                    A("dve", lambda e: e.tensor_copy(out=hv[:, :, :], in_=v3(PB[4][:, :])), r=[pbn(4)], w=[hn])
                    if h + 1 < 4:
                        hslots.append(load_head(h + 1))

                def gates(h):
                    p_ = h % 2
                    QF, SG, GS = (5, 0)[p_], (6, 1)[p_], (7, 2)[p_]
                    hv = hiT if p_ == 0 else hiT2
                    hn = "hiT" if p_ == 0 else "hiT2"
                    A("dve", lambda e, h=h: e.tensor_scalar(T(SG), T(SG), oml[:, h:h + 1], lb[:, h:h + 1], op0=ALU.mult, op1=ALU.add),
                      r=[Tn(SG), "oml", "lb"], w=[Tn(SG)])
                    A("act", lambda e: e.activation(out=T(LOGF), in_=T(SG), func=AF.Ln), r=[Tn(SG)], w=[Tn(LOGF)])
                    A("dve", lambda e: e.tensor_scalar(T(SG), T(SG), -1.0, 1.0, op0=ALU.mult, op1=ALU.add), r=[Tn(SG)], w=[Tn(SG)])
                    for c in range(8):
                        A("dve", lambda e, c=c: e.tensor_tensor_scan(out=TS[:, BC, c * 64:(c + 1) * 64], data0=onesf[:, 0:64],
                                                                     data1=TS[:, LOGF, c * 64:(c + 1) * 64], initial=0.0, op0=ALU.mult, op1=ALU.add),
                          r=[Tn(LOGF), "onesf"], w=[Tn(BC)])
                    A("act", lambda e: e.activation(out=T(LOGF), in_=T(BC), func=AF.Exp), r=[Tn(BC)], w=[Tn(LOGF)])
                    A("pool", lambda e: e.tensor_tensor(out=BS[:, QE, :], in0=T(QF), in1=T(LOGF), op=ALU.mult), r=[Tn(QF), Tn(LOGF)], w=["QE"])
                    A("dve", lambda e: e.tensor_tensor(out=v3(T(DQ), 8), in0=v3(T(BC), 8),
                                                       in1=v3(T(BC), 8)[:, :, 63:64].to_broadcast([128, 8, 64]), op=ALU.subtract),
                      r=[Tn(BC)], w=[Tn(DQ)])
                    A("act", lambda e: e.activation(out=T(EQ), in_=T(DQ), func=AF.Exp, scale=-1.0), r=[Tn(DQ)], w=[Tn(EQ)])
                    A("pool", lambda e: e.tensor_tensor(out=BS[:, KD, :], in0=T(SG), in1=T(EQ), op=ALU.mult), r=[Tn(SG), Tn(EQ)], w=["KD"])
                    for i in range(4):
                        A("pe", lambda e, i=i: e.transpose(out=PBh[:, i * 128:(i + 1) * 128], in_=BS[:, KD, i * 128:(i + 1) * 128], identity=identb[:, :]),
                          r=["KD", "identb"], w=["pbh"])
                    A("act", lambda e: e.copy(out=KDT[:, :, :], in_=v3(PBh[:, 0:512])), r=["pbh"], w=["KDT"])

                def levels_chain(h):
                    p_ = h % 2
                    QF, SG, GS = (5, 0)[p_], (6, 1)[p_], (7, 2)[p_]
                    hv = hiT if p_ == 0 else hiT2
                    hn = "hiT" if p_ == 0 else "hiT2"
                    LBK = (0, 1, 2, 4)
                    for l, (BL, off) in enumerate(((8, 0), (16, 8), (32, 16), (64, 32))):
                        nb = 512 // BL
                        A("dve", lambda e, BL=BL, off=off, nb=nb: e.tensor_tensor(
                            out=T(DQ).rearrange("p (n j) -> p n j", j=BL), in0=T(BC).rearrange("p (n j) -> p n j", j=BL),
                            in1=T(BC).rearrange("p (n j) -> p n j", j=BL)[:, :, off:off + 1].to_broadcast([128, nb, BL]), op=ALU.subtract),
                          r=[Tn(BC)], w=[Tn(DQ)])
                        if l == 0:
                            A("act", lambda e: e.activation(out=T(EQ), in_=T(DQ), func=AF.Exp), r=[Tn(DQ)], w=[Tn(EQ)])
                            A("act", lambda e: e.activation(out=T(EK), in_=T(DQ), func=AF.Exp, scale=-1.0), r=[Tn(DQ)], w=[Tn(EK)])
                        else:
                            A("dve", lambda e: e.tensor_scalar(T(EQ), T(DQ), 0.0, None, op0=ALU.min), r=[Tn(DQ)], w=[Tn(EQ)])
                            A("dve", lambda e: e.tensor_scalar(T(EK), T(DQ), 0.0, None, op0=ALU.max), r=[Tn(DQ)], w=[Tn(EK)])
                            A("act", lambda e: e.activation(out=T(EQ), in_=T(EQ), func=AF.Exp), r=[Tn(EQ)], w=[Tn(EQ)])
                            A("act", lambda e: e.activation(out=T(EK), in_=T(EK), func=AF.Exp, scale=-1.0), r=[Tn(EK)], w=[Tn(EK)])
                        A("pool", lambda e: e.tensor_tensor(out=BS[:, QL, :], in0=T(QF), in1=T(EQ), op=ALU.mult), r=[Tn(QF), Tn(EQ)], w=["QL"])
                        A("dve", lambda e: e.tensor_tensor(out=BS[:, KL, :], in0=T(SG), in1=T(EK), op=ALU.mult), r=[Tn(SG), Tn(EK)], w=["KL"])
                        for i in range(4):
                            A("pe", lambda e, i=i, l=l: e.matmul(PB[LBK[i]][:, l * 128:(l + 1) * 128], lhsT=BS[:, KL, i * 128:(i + 1) * 128],
                                                                  rhs=BS[:, QL, i * 128:(i + 1) * 128], start=True, stop=True),
                              r=["KL", "QL"], w=[pbn(LBK[i])])
                    for i in range(4):
                        A("dve", lambda e, i=i: e.tensor_tensor(out=T(XS), in0=PB[LBK[i]][:, :], in1=cstt[:, 128:640], op=ALU.mult),
                          r=[pbn(LBK[i]), "cst"], w=[Tn(XS)])
                        A("dve", lowp(lambda e, i=i: e.tensor_reduce(out=SC[:, i, :], in_=T(XS).rearrange("p (l t) -> p t l", l=4), axis=AX.X, op=ALU.add)),
                          r=[Tn(XS)], w=["SC%d" % i])
                    A("act", lambda e, h=h: e.copy(out=SBF[:, 0, :], in_=statebf[:, h, :]), r=["sbf%d" % h], w=["SBF0"])
                    for c in range(8):
                        i, hh = divmod(c, 2)
                        pbk = 3 if hh == 0 else 5
                        A("pe", lambda e, i=i, hh=hh, c=c, pbk=pbk: e.matmul(PB[pbk][:, (c // 2) * 128:(c // 2 + 1) * 128],
                                                                             lhsT=KDT[hh * 64:(hh + 1) * 64, i, :],
                                                                             rhs=hv[hh * 64:(hh + 1) * 64, i, :], start=True, stop=True),
                          r=["KDT", hn], w=[pbn(pbk)])
                    for c in range(8):
                        pbk = 3 if c % 2 == 0 else 5
                        A("dve", lambda e, c=c, h=h, pbk=pbk: e.scalar_tensor_tensor(
                            out=state[:, h, :], in0=state[:, h, :], scalar=TS[:, LOGF, c * 64 + 63:c * 64 + 64],
                            in1=PB[pbk][:, (c // 2) * 128:(c // 2 + 1) * 128], op0=ALU.mult, op1=ALU.add),
                          r=["st%d" % h, Tn(LOGF), pbn(pbk)], w=["st%d" % h])
                        if c < 7:
                            A("act", lambda e, c=c, h=h: e.copy(out=SBF[:, c + 1, :], in_=state[:, h, :]), r=["st%d" % h], w=["SBF%d" % (c + 1)])
                        else:
                            A("act", lambda e, h=h: e.copy(out=statebf[:, h, :], in_=state[:, h, :]), r=["st%d" % h], w=["sbf%d" % h])
                    for i in range(4):
                        A("pe", lambda e, i=i: e.matmul(PB[6][:, i * 128:(i + 1) * 128], lhsT=hv[:, i, :], rhs=SC[:, i, :], start=True, stop=False),
                          r=[hn, "SC%d" % i], w=[pbn(6)])
                        for hh in range(2):
                            c = 2 * i + hh
                            A("pe", lambda e, i=i, hh=hh, c=c: e.matmul(PB[6][:, i * 128 + hh * 64:i * 128 + (hh + 1) * 64], lhsT=SBF[:, c, :],
                                                                        rhs=BS[:, QE, c * 64:(c + 1) * 64], start=False, stop=(hh == 1)),
                              r=["SBF%d" % c, "QE"], w=[pbn(6)])

                def norm(h):
                    p_ = h % 2
                    GS = (7, 2)[p_]
                    A("act", lambda e: e.activation(out=T(OSQ), in_=PB[6][:, :], func=AF.Square), r=[pbn(6)], w=[Tn(OSQ)])
                    A("dve", lambda e: e.tensor_copy(out=T(OSB), in_=PB[6][:, :]), r=[pbn(6)], w=[Tn(OSB)])
                    A("pe", lambda e: e.matmul(PB[5][:, :], lhsT=onesf[:, :], rhs=T(OSQ), start=True, stop=True), r=["onesf", Tn(OSQ)], w=[pbn(5)])
                    A("act", lambda e: e.activation(out=T(RSB), in_=PB[5][:, :], func=AF.Ln, scale=1.0 / 128, bias=EPS), r=[pbn(5)], w=[Tn(RSB)])
                    A("act", lambda e: e.activation(out=T(RSB), in_=T(RSB), func=AF.Exp, scale=-0.5), r=[Tn(RSB)], w=[Tn(RSB)])
                    A("dve", lambda e: e.tensor_tensor(out=T(OSB), in0=T(OSB), in1=T(RSB), op=ALU.mult), r=[Tn(OSB), Tn(RSB)], w=[Tn(OSB)])
                    A("pool", lambda e, h=h: e.tensor_tensor(out=mTh[:, h, :], in0=T(OSB), in1=T(GS), op=ALU.mult), r=[Tn(OSB), Tn(GS)], w=["mTh"])

                def lane_h0():
                    inproj(0)
                    gates(0)
                weave(record(_attention), record(lane_h0))
                for h in range(4):
                    if h == 0:
                        inproj(1)
                    levels_chain(h)
                    if h + 1 < 4:
                        def lane_a(h=h):
                            norm(h)
                            if h + 2 < 4:
                                inproj(h + 2)
                        weave(record(lane_a), record(gates, h + 1))
                    else:
                        norm(h)


            _hgrn()
            if "aT" in dbg_out:
                dma("sp", dbg_out["aT"][s], aT[:, :, :], r=["aT"], key="dbgA%d" % s)
                dma("sp", dbg_out["mTh"][s], mTh[:, :, :], r=["mTh"], key="dbgM%d" % s)
            for i in range(4):
                lt = lt0 + i
                for dh in range(2):
                    pb = dh
                    mms = [(aT[:, c, i * 128:(i + 1) * 128], Wd[:, c, dh * 512:(dh + 1) * 512], ["aT", "Wd0"]) for c in range(4)]
                    mms += [(mTh[:, h, i * 128:(i + 1) * 128], Wd[:, 4 + h, dh * 512:(dh + 1) * 512], ["mTh", "Wd1"]) for h in range(4)]
                    mms += [(onesb[0:1, :], rows[0:1, dh * 512:(dh + 1) * 512], ["onesb", "rows0"])]
                    for n_, (l_, r_, res) in enumerate(mms):
                        A("pe", lambda e, l_=l_, r_=r_, pb=pb, st_=(n_ == 0), sp_=(n_ == len(mms) - 1): e.matmul(PB[pb][:, :], lhsT=l_, rhs=r_, start=st_, stop=sp_),
                          r=res, w=[pbn(pb)])
                    A("dve", lambda e, pb=pb, dh=dh: e.tensor_tensor(out=T(3 + dh), in0=PB[pb][:, :], in1=gt1bc[:, dh * 512:(dh + 1) * 512], op=ALU.mult),
                      r=[pbn(pb), "gt1bc"], w=[Tn(3 + dh)])
                    A("pool", lambda e, lt=lt, dh=dh: e.tensor_tensor(out=xg[:, lt, dh * 512:(dh + 1) * 512], in0=T(3 + dh),
                                                                      in1=xg[:, lt, dh * 512:(dh + 1) * 512], op=ALU.add),
                      r=[Tn(3 + dh), "xg%d" % lt], w=["xg%d" % lt])
                if "x1" in dbg_out:
                    dma("sp", dbg_out["x1"][s * 512 + i * 128:s * 512 + (i + 1) * 128, :], xg[:, lt, :], r=["xg%d" % lt], key="dbgX%d" % lt)

        LG, M8, MSK, NM, EX, SMC = rt[:, 0:32], rt[:, 32:40], rt[:, 40:72], rt[:, 72:73], rt[:, 80:112], rt[:, 73:74]

        def router(g, lt):
            xt = xg[:, lt, :]
            norm_T(lt, 16, lt)
            A("act", lambda e: e.activation(out=SQ[:, :], in_=xt, func=AF.Copy, scale=rstd), r=["xg%d" % lt, "rstd"], w=["SQ"])
            for kc in range(8):
                pbi = 2 + kc // 4
                A("pe", lambda e, kc=kc, pbi=pbi: e.transpose(out=PB[pbi][:, (kc % 4) * 128:(kc % 4 + 1) * 128], in_=SQ[:, kc * 128:(kc + 1) * 128],
                                                              identity=ident32), r=["SQ", "cst"], w=[pbn(pbi)])
            H32 = TS[:, 0:2, :].rearrange("p a (b c) -> p (a b) c", c=128)
            for kc in range(8):
                pbi = 2 + kc // 4
                A("act", lambda e, kc=kc, pbi=pbi: e.activation(out=H32[:, kc, :], in_=PB[pbi][:, (kc % 4) * 128:(kc % 4 + 1) * 128], func=AF.Identity,
                                                                scale=AB[:, 16 + kc:17 + kc], bias=AB[:, 24 + kc:25 + kc]),
                  r=[pbn(pbi), "AB"], w=[Tn(kc // 4)])
            for kc in range(8):
                A("pe", lambda e, kc=kc: e.matmul(PB[4][:, 0:NE], lhsT=H32[:, kc, :], rhs=wr[:, kc, :], start=(kc == 0), stop=(kc == 7)),
                  r=[Tn(kc // 4), "wr"], w=[pbn(4)])
            A("dve", lambda e: e.tensor_tensor(out=LG, in0=PB[4][:, 0:NE], in1=brbc[:, :], op=ALU.add), r=[pbn(4), "brbc"], w=["LG"])
            A("dve", lambda e: e.max(out=M8, in_=LG), r=["LG"], w=["M8"])
            A("dve", lambda e: e.tensor_scalar(MSK, LG, M8[:, 3:4], None, op0=ALU.is_ge), r=["LG", "M8"], w=["MSK"])
            A("dve", lambda e: e.tensor_scalar(NM, M8[:, 0:1], -1.0, None, op0=ALU.mult), r=["M8"], w=["NM"])
            A("act", lambda e: e.activation(out=EX, in_=LG, func=AF.Exp, bias=NM), r=["LG", "NM"], w=["EX"])
            A("dve", lambda e: e.tensor_tensor(out=EX, in0=EX, in1=MSK, op=ALU.mult), r=["EX", "MSK"], w=["EX"])
            A("dve", lambda e: e.reduce_sum(out=SMC, in_=EX, axis=AX.X), r=["EX"], w=["SMC"])
            A("dve", lambda e: e.reciprocal(out=SMC, in_=SMC), r=["SMC"], w=["SMC"])
            A("dve", lambda e, lt=lt: e.tensor_scalar(comb[:, lt, :], EX, SMC, None, op0=ALU.mult), r=["EX", "SMC"], w=["comb%d" % lt])
            if "comb" in dbg_out:
                dma("sp", dbg_out["comb"][g * 1024 + lt * 128:g * 1024 + (lt + 1) * 128, :], comb[:, lt, :], r=["comb%d" % lt], key="dbgC%d" % lt)

        def routerB(lt):
            A("pe", lambda e, lt=lt: e.transpose(out=PB[5][0:32, 0:128], in_=comb[:, lt, :], identity=ident32), r=["comb%d" % lt, "cst"], w=[pbn(5)])
            A("act", lambda e: e.copy(out=CT[:, :], in_=PB[5][0:32, 0:128]), r=[pbn(5)], w=["CT"])
            for dh in range(2):
                A("pe", lambda e, dh=dh: e.matmul(PB[dh][:, :], lhsT=CT[:, :], rhs=bdn[:, dh * 512:(dh + 1) * 512], start=True, stop=True),
                  r=["CT", "bdn"], w=[pbn(dh)])
                A("dve", lambda e, dh=dh: e.tensor_tensor(out=T(3 + dh), in0=PB[dh][:, :], in1=gt2bc[:, dh * 512:(dh + 1) * 512], op=ALU.mult),
                  r=[pbn(dh), "gt2bc"], w=[Tn(3 + dh)])
                A("pool", lambda e, lt=lt, dh=dh: e.tensor_tensor(out=xg[:, lt, dh * 512:(dh + 1) * 512], in0=T(3 + dh),
                                                                  in1=xg[:, lt, dh * 512:(dh + 1) * 512], op=ALU.add),
                  r=[Tn(3 + dh), "xg%d" % lt], w=["xg%d" % lt])

        def moe(g):
            pieces = [(e_, pc) for e_ in range(NE) for pc in range(4)]
            slots = {}

            def load_piece(n):
                e_, pc = pieces[n]
                k = ring_slot()
                slots[n] = k
                dma("pool", ring[:, k, :, :], w_gu[e_, :, pc * 512:(pc + 1) * 512].rearrange("(kc p) n -> p kc n", p=128),
                    w=rres(k), key="ring%d.0" % k)

            def load_wd(e_):
                for hf in range(2):
                    dma("pool", Wd[:, hf * 4:(hf + 1) * 4, :], w_dn[e_, hf * 512:(hf + 1) * 512, :].rearrange("(kc p) n -> p kc n", p=128),
                        w=["Wd%d" % hf], key="Wd%d" % hf)

            for n in range(3):
                load_piece(n)
            load_wd(0)
            cnt = 0
            cnt2 = 0
            pend_e = []

            def flush_e():
                while pend_e:
                    U1, T1, ffc, half = pend_e.pop(0)
                    A("dve", lambda e, U1=U1, T1=T1, ffc=ffc, half=half: e.scalar_tensor_tensor(
                        out=actT[:, ffc, half * 512:(half + 1) * 512], in0=T(U1), scalar=8.0, in1=T(T1), op0=ALU.min, op1=ALU.mult),
                      r=[Tn(U1), Tn(T1)], w=["act%d_%d" % (ffc, half)])
            for e_ in range(NE):
                for pc in range(4):
                    n = e_ * 4 + pc
                    k = slots[n]
                    for half in range(2):
                        hres = ["hT%d" % t for t in range(half * 4, half * 4 + 4)]
                        for jj in range(2):
                            ffc = pc * 2 + jj
                            par = cnt % 2
                            cnt += 1
                            pg, pu = par, 2 + par
                            GC, SGM, U1, T1 = 5 + par * 4, 6 + par * 4, 7 + par * 4, 8 + par * 4
                            for kc in range(8):
                                A("pe", lambda e, kc=kc, k=k, jj=jj, pg=pg, half=half: e.matmul(
                                    PB[pg][:, :], lhsT=ring[:, k, kc, jj * 256:jj * 256 + 256:2], rhs=hT[:, kc, half * 512:(half + 1) * 512],
                                    start=(kc == 0), stop=(kc == 7)), r=rres(k) + hres, w=[pbn(pg)])
                            for kc in range(8):
                                A("pe", lambda e, kc=kc, k=k, jj=jj, pu=pu, half=half: e.matmul(
                                    PB[pu][:, :], lhsT=ring[:, k, kc, jj * 256 + 1:jj * 256 + 256:2], rhs=hT[:, kc, half * 512:(half + 1) * 512],
                                    start=(kc == 0), stop=(kc == 7)), r=rres(k) + hres, w=[pbn(pu)])
                            bcol = PV_BGU + e_ * 16 + ffc * 2
                            A("dve", lambda e, pg=pg, GC=GC, bcol=bcol: e.tensor_scalar(T(GC), PB[pg][:, :], pv[:, bcol:bcol + 1], 7.0, op0=ALU.add, op1=ALU.min),
                              r=[pbn(pg), "pv"], w=[Tn(GC)])
                            A("act", lambda e, GC=GC, SGM=SGM: e.activation(out=T(SGM), in_=T(GC), func=AF.Sigmoid, scale=1.702), r=[Tn(GC)], w=[Tn(SGM)])
                            A("dve", lambda e, pu=pu, U1=U1, bcol=bcol: e.tensor_scalar(T(U1), PB[pu][:, :], pv[:, bcol + 1:bcol + 2], -6.0, op0=ALU.add, op1=ALU.max),
                              r=[pbn(pu), "pv"], w=[Tn(U1)])
                            A("pool", lambda e, GC=GC, SGM=SGM, T1=T1: e.tensor_tensor(out=T(T1), in0=T(GC), in1=T(SGM), op=ALU.mult),
                              r=[Tn(GC), Tn(SGM)], w=[Tn(T1)])
                            flush_e()
                            pend_e.append((U1, T1, ffc, half))
                    if n + 3 < len(pieces):
                        load_piece(n + 3)
                for lt in range(8):
                    ares = ["act%d_%d" % (kc, lt // 4) for kc in range(8)]
                    for dh in range(2):
                        py = 4 + cnt2 % 2
                        tm = 3 + cnt2 % 2
                        cnt2 += 1
                        for kc in range(8):
                            A("pe", lambda e, kc=kc, lt=lt, dh=dh, py=py: e.matmul(PB[py][:, :], lhsT=actT[:, kc, lt * 128:(lt + 1) * 128],
                                                                                    rhs=Wd[:, kc, dh * 512:(dh + 1) * 512], start=(kc == 0), stop=(kc == 7)),
                              r=ares + ["Wd0", "Wd1"], w=[pbn(py)])
                        A("dve", lambda e, py=py, tm=tm, dh=dh: e.tensor_tensor(out=T(tm), in0=PB[py][:, :], in1=gt2bc[:, dh * 512:(dh + 1) * 512], op=ALU.mult),
                          r=[pbn(py), "gt2bc"], w=[Tn(tm)])
                        A("dve", lambda e, tm=tm, lt=lt, dh=dh, e_=e_: e.scalar_tensor_tensor(
                            out=xg[:, lt, dh * 512:(dh + 1) * 512], in0=T(tm), scalar=comb[:, lt, e_:e_ + 1], in1=xg[:, lt, dh * 512:(dh + 1) * 512],
                            op0=ALU.mult, op1=ALU.add), r=[Tn(tm), "comb%d" % lt, "xg%d" % lt], w=["xg%d" % lt])
                        if lt == 1 and dh == 1:
                            flush_e()
                    if e_ == NE - 1 and lt >= 2:
                        final(g, [lt - 2])
                if e_ + 1 < NE:
                    load_wd(e_ + 1)

        def final(g, lts=range(8)):
            for lt in lts:
                norm_stats(lt)
                ob = lt % 2
                OUTT = TS[:, 11 + 2 * ob:13 + 2 * ob, :].rearrange("p a b -> p (a b)")
                ores = [Tn(11 + 2 * ob), Tn(12 + 2 * ob)]
                A("dve", lambda e, lt=lt, OUTT=OUTT: e.scalar_tensor_tensor(out=OUTT, in0=xg[:, lt, :], scalar=rstd, in1=gfbc[:, :],
                                                                            op0=ALU.mult, op1=ALU.mult),
                  r=["xg%d" % lt, "rstd", "gfbc"], w=ores)
                dma("sp", out[g * 1024 + lt * 128:g * 1024 + (lt + 1) * 128, :], OUTT, r=ores, key="o%d" % ob)

        for g in range(NGRP):
            for c in range(4):
                for gq in range(2):
                    r0 = (gq * 4 + c) * 64
                    dma("pool", Wd[gq * 64:(gq + 1) * 64, c, :], w_out[r0:r0 + 64, :], w=["Wd0"], key="Wo")
            dma("pool", Wd[:, 4:8, :], w_out[512:1024, :].rearrange("(h p) n -> p h n", p=128), w=["Wd1"], key="Wd1")
            phase1(g, 0)
            phase1(g, 1)
            router(g, 0)
            for lt in range(7):
                weave(record(router, g, lt + 1), record(routerB, lt))
            routerB(7)
            moe(g)
            final(g, [6, 7])
        S.emit(nc)
    return nc


def _t5_bucket(dist):
    n = np.maximum(dist, 0)
    nf = np.maximum(n, 1).astype(np.float32)
    large = 16 + (np.log(nf / np.float32(16)) / np.float32(math.log(128 / 16)) * np.float32(16)).astype(np.int32)
    large = np.minimum(large, 31)
    return np.where(n < 16, n, large)


def _static_tables():
    oh = np.zeros((32, 2, 255), np.float32)
    neg = np.zeros((8, 2, 255), np.float32)
    for u in range(255):
        if u <= 127:
            oh[_t5_bucket(np.array(u)), 1, u] = 1.0
            neg[:, 0, u] = NEG
        else:
            oh[_t5_bucket(np.array(u - 127)), 0, u] = 1.0
            neg[:, 1, u] = NEG
    cst = np.zeros((128, 640), np.float32)
    cst[:, 0:128] = np.eye(128, dtype=np.float32)
    s = np.arange(128)[:, None]
    t = np.arange(128)[None, :]
    cst[:, 128:256] = ((s // 8 == t // 8) & (t >= s)).astype(np.float32)
    for li, BL in enumerate((16, 32, 64)):
        cst[:, 256 + li * 128:384 + li * 128] = ((s // BL == t // BL) & (t % BL >= BL // 2) & (s % BL < BL // 2)).astype(np.float32)
    return oh.reshape(32, 510), neg.reshape(8, 510), cst


def _pack_pvec(b, c, g_mix, b_ada, b_in, g_ffn, hg_lb, hg_norm_w, attn_sinks, b_gate_up):
    pvv = np.zeros((128, NPV), np.float32)
    pvv[:, PV_C:PV_C + 8] = c[b].reshape(8, 128).T
    pvv[:, PV_GMIX:PV_GMIX + 8] = g_mix[0].reshape(8, 128).T
    pvv[:, PV_GFFN:PV_GFFN + 8] = g_ffn[0].reshape(8, 128).T
    pvv[:, PV_BADA:PV_BADA + 48] = b_ada[0].reshape(48, 128).T
    pvv[:, PV_BIN:PV_BIN + 22] = b_in[0].reshape(22, 128).T
    bq = b_in[0][0:512].reshape(8, 64)
    for cc in range(4):
        pvv[0:64, PV_BQ + cc] = bq[cc]
        pvv[64:128, PV_BQ + cc] = bq[4 + cc]
    pvv[:, PV_LB:PV_LB + 8] = hg_lb.reshape(2, 4, 128).transpose(2, 0, 1).reshape(128, 8)
    pvv[:, PV_NW] = hg_norm_w[0]
    pvv[0:64, PV_SINK:PV_SINK + 4] = attn_sinks[0][None, 0:4]
    pvv[64:128, PV_SINK:PV_SINK + 4] = attn_sinks[0][None, 4:8]
    pvv[:, PV_BGU:PV_BGU + 512] = b_gate_up[0].reshape(32, 8, 128, 2).transpose(2, 0, 1, 3).reshape(128, 512)
    return pvv


_NC_CACHE = {}


def run(inputs, SEQ=4096, cores=8, dbg=()):
    f = lambda a: np.ascontiguousarray(np.asarray(a, dtype=np.float32))
    x = f(inputs["x"])
    oh, neg, cst = _static_tables()
    b_in = f(inputs["b_in"])
    rowsin = np.zeros((2, D), np.float32)
    rowsin[0] = f(inputs["b_out"])[0]
    rowsin[1, 0:128] = b_in[0][640:768]
    rowsin[1, 128:640] = b_in[0][1792:2304]
    shared = dict(
        w_ada=f(inputs["w_ada"])[0], w_in=f(inputs["w_in"])[0], rowsin=rowsin, relb=f(inputs["rel_bias"]), ohb=oh, negb=neg, cst=cst,
        w_out=f(inputs["w_out"])[0], w_router=f(inputs["w_router"])[0], brt=f(inputs["b_router"]).reshape(1, NE),
        w_gu=f(inputs["w_gate_up"])[0], w_dn=f(inputs["w_down"])[0], b_dn=f(inputs["b_down"])[0], gfin=f(inputs["g_final"]).reshape(1, D))
    in_maps = []
    for b in range(cores):
        m = dict(shared)
        m["x"] = np.ascontiguousarray(x[b, :SEQ])
        m["pvec"] = _pack_pvec(b, f(inputs["c"]), f(inputs["g_mix"]), f(inputs["b_ada"]), b_in, f(inputs["g_ffn"]), f(inputs["hg_lb"]),
                               f(inputs["hg_norm_w"]), f(inputs["attn_sinks"]), f(inputs["b_gate_up"]))
        in_maps.append(m)
    key = (SEQ, tuple(dbg))
    if key not in _NC_CACHE:
        _NC_CACHE[key] = build(SEQ, dbg)
    res = run_bass_kernel_spmd(_NC_CACHE[key], in_maps, core_ids=list(range(cores)))
    return res


def kernel(**inputs):
    res = run(inputs, SEQ=4096, cores=8)
    return np.stack([np.asarray(r["out"], dtype=np.float32) for r in res.results], axis=0)
```

```python
import contextlib
import math

import numpy as np
import concourse.bass as bass
import concourse.mybir as mybir
from concourse.bass_utils import run_bass_kernel_spmd

F32 = mybir.dt.float32
BF16 = mybir.dt.bfloat16
AF = mybir.ActivationFunctionType
ALU = mybir.AluOpType
AX = mybir.AxisListType

D = 1024
NE = 32
EPS = 1e-5
ATT_SCALE = 64 ** -0.5
IN_COLS = 2816
NEG = -30000.0

PV_C, PV_GMIX, PV_GFFN, PV_BADA, PV_BIN, PV_BQ, PV_LB, PV_NW, PV_SINK, PV_BGU = 0, 8, 16, 24, 72, 94, 98, 106, 107, 111
NPV = PV_BGU + 512


class _Op:
    __slots__ = ("stream", "fn", "dma", "deps", "sig", "tok")


class Sched:
    STREAMS = ("pe", "act", "dve", "pool", "sp")

    def __init__(self):
        self.ops = {s: [] for s in self.STREAMS}
        self.last_write = {}
        self.readers = {}
        self.dma_counts = {}

    def add(self, stream, fn, r=(), w=(), dma=None):
        op = _Op()
        op.stream, op.fn, op.dma, op.sig, op.tok = stream, fn, dma is not None, False, None
        pr = [x for x in r if x.startswith("pb")]
        if pr:
            r = [x for x in r if not x.startswith("pb")]
            w = list(w) + pr
        deps = {}
        for x in r:
            d = self.last_write.get(x)
            if d is not None:
                deps[id(d)] = (d, True)
        for x in w:
            d = self.last_write.get(x)
            if d is not None and id(d) not in deps:
                deps[id(d)] = (d, False)
            for rd in self.readers.get(x, ()):
                if id(rd) not in deps:
                    deps[id(rd)] = (rd, False)
        op.deps = []
        for d, raw in deps.values():
            if d.dma or d.stream != stream or op.dma:
                need = True
            else:
                need = stream != "pe"
            if need:
                op.deps.append(d)
                d.sig = True
        for x in r:
            self.readers.setdefault(x, []).append(op)
        for x in w:
            self.last_write[x] = op
            self.readers[x] = []
        if op.dma:
            c = self.dma_counts.get(dma, 0) + 16
            self.dma_counts[dma] = c
            op.tok = (("dma", dma), c)
        self.ops[stream].append(op)
        return op

    def emit(self, nc):
        for s in self.STREAMS:
            c = 0
            for op in self.ops[s]:
                if not op.dma and op.sig:
                    c += 1
                    op.tok = (("eng", s), c)
        keys = [("eng", s) for s in self.STREAMS] + [("dma", k) for k in self.dma_counts]
        with contextlib.ExitStack() as es:
            sems = {}
            for k in keys:
                sems[k] = es.enter_context(nc.semaphore("s_%s_%s" % (k[0], k[1])))
            block = es.enter_context(nc.Block())
            sched = self

            def run_stream(s, eng):
                waited = {}
                for op in sched.ops[s]:
                    need = {}
                    for d in op.deps:
                        k, v = d.tok
                        if k[0] == "dma" and k[1].startswith("T:"):
                            v = sched.dma_counts[k[1]]
                        if need.get(k, 0) < v:
                            need[k] = v
                    for k, v in need.items():
                        if waited.get(k, 0) < v:
                            eng.wait_ge(sems[k], v)
                            waited[k] = v
                    ins = op.fn(eng)
                    if op.dma:
                        ins.then_inc(sems[op.tok[0]], 16)
                    elif op.sig:
                        ins.then_inc(sems[op.tok[0]], 1)
                if s == "sp":
                    for k, c in sched.dma_counts.items():
                        if waited.get(("dma", k), 0) < c:
                            eng.wait_ge(sems[("dma", k)], c)

            @block.tensor
            def _(e):
                run_stream("pe", e)

            @block.scalar
            def _(e):
                run_stream("act", e)

            @block.vector
            def _(e):
                run_stream("dve", e)

            @block.gpsimd
            def _(e):
                run_stream("pool", e)

            @block.sync
            def _(e):
                run_stream("sp", e)


def build(SEQ=4096, dbg=()):
    nc = bass.Bass("TRN2", target_bir_lowering=False)
    NGRP = SEQ // 1024

    def din(name, shape):
        return nc.dram_tensor(name, list(shape), F32, kind="ExternalInput").ap()

    x = din("x", [SEQ, D])
    pvec = din("pvec", [128, NPV])
    w_ada = din("w_ada", [D, 6 * D])
    w_in = din("w_in", [D, IN_COLS])
    rowsin = din("rowsin", [2, D])
    relb = din("relb", [32, 8])
    ohb = din("ohb", [32, 510])
    negb = din("negb", [8, 510])
    cst = din("cst", [128, 640])
    w_out = din("w_out", [D, D])
    w_router = din("w_router", [D, NE])
    brt = din("brt", [1, NE])
    w_gu = din("w_gu", [NE, D, 2 * D])
    w_dn = din("w_dn", [NE, D, D])
    b_dn = din("b_dn", [NE, D])
    gfin = din("gfin", [1, D])
    out = nc.dram_tensor("out", [SEQ, D], F32, kind="ExternalOutput").ap()
    scr = nc.dram_tensor("scr", [8, 2, 129 * 255], F32, kind="Internal").ap()
    dbg_out = {}
    if "x1" in dbg:
        dbg_out["x1"] = nc.dram_tensor("dbg_x1", [SEQ, D], F32, kind="ExternalOutput").ap()
    if "comb" in dbg:
        dbg_out["comb"] = nc.dram_tensor("dbg_comb", [SEQ, NE], F32, kind="ExternalOutput").ap()
    if "mix" in dbg:
        dbg_out["aT"] = nc.dram_tensor("dbg_aT", [SEQ // 512, 128, 4, 512], BF16, kind="ExternalOutput").ap()
        dbg_out["mTh"] = nc.dram_tensor("dbg_mTh", [SEQ // 512, 128, 4, 512], BF16, kind="ExternalOutput").ap()

    S = Sched()
    lane_stack = []

    def A(stream, fn, r=(), w=(), dma=None):
        if lane_stack:
            lane_stack[-1].append((stream, fn, tuple(r), tuple(w), dma))
        else:
            S.add(stream, fn, r=r, w=w, dma=dma)

    def record(f, *args):
        lane_stack.append([])
        f(*args)
        return lane_stack.pop()

    def weave(*lanes):
        chunks = []
        for ln in lanes:
            cl = []
            for op in ln:
                if cl and op[0] == "pe" and cl[-1][-1][0] == "pe":
                    cl[-1].append(op)
                else:
                    cl.append([op])
            chunks.append(cl)
        tot = [max(1, sum(len(c) for c in cl)) for cl in chunks]
        done = [0] * len(lanes)
        pos = [0] * len(lanes)
        while True:
            best = None
            for li, cl in enumerate(chunks):
                if pos[li] < len(cl):
                    fr = done[li] / tot[li]
                    if best is None or fr < best[0]:
                        best = (fr, li)
            if best is None:
                break
            li = best[1]
            for (stream, fn, r, w, dm) in chunks[li][pos[li]]:
                A(stream, fn, r=r, w=w, dma=dm)
            done[li] += len(chunks[li][pos[li]])
            pos[li] += 1
    es = contextlib.ExitStack()
    with es:
        def sb(name, shape, dt=F32):
            return es.enter_context(nc.sbuf_tensor(name, list(shape), dt))

        def psum(name, shape, dt=F32):
            return es.enter_context(nc.psum_tensor(name, list(shape), dt))

        xg = sb("xg", [128, 8, D])
        hT = sb("hT", [128, 8, 1024], BF16)
        actT = sb("actT", [128, 8, 1024], BF16)
        ring = sb("ring", [128, 3, 8, 512], BF16)
        Wd = sb("Wd", [128, 8, 1024], BF16)
        NTS = 15
        TS = sb("TS", [128, NTS, 512])
        SQ = sb("SQ", [128, D])
        E = sb("E", [128, 2, 8, 128])
        gt1bc = sb("gt1bc", [128, D])
        gt2bc = sb("gt2bc", [128, D])
        gfbc = sb("gfbc", [128, D])
        pv = sb("pv", [128, NPV])
        rows = sb("rows", [33, D], BF16)
        bdn = sb("bdn", [32, D])
        wr = sb("wr", [128, 8, NE])
        brbc = sb("brbc", [128, NE])
        cstt = sb("cstt", [128, 640])
        identb = sb("identb", [128, 128], BF16)
        onesf = sb("onesf", [128, 128])
        onesb = sb("onesb", [128, 128], BF16)
        modT = sb("modT", [128, 48])
        AB = sb("AB", [128, 32])
        sm = sb("sm", [128, 64])
        relbt = sb("relbt", [32, 8])
        xn = sb("xn", [128, 2, D], BF16)
        qT = sb("qT", [128, 4, 4, 128], BF16)
        kT = sb("kT", [128, 640], BF16)
        vA = sb("vA", [128, 5, 128], BF16)
        aT = sb("aT", [128, 4, 512], BF16)
        mTh = sb("mTh", [128, 4, 512], BF16)
        hiT = sb("hiT", [128, 4, 128], BF16)
        hiT2 = sb("hiT2", [128, 4, 128], BF16)
        KDT = sb("KDT", [128, 4, 128], BF16)
        SC = sb("SC", [128, 4, 128], BF16)
        BS = sb("BS", [128, 6, 512], BF16)
        state = sb("state", [128, 4, 128])
        statebf = sb("statebf", [128, 4, 128], BF16)
        SBF = sb("SBF", [128, 8, 128], BF16)
        comb = sb("comb", [128, 8, NE])
        CT = sb("CT", [32, 128])
        rt = sb("rt", [128, 160])
        PB = [psum("pb%d" % i, [128, 512]) for i in range(7)]
        PBh = psum("pbh", [128, 1024], BF16)

        def pbn(i):
            return "pb%d" % i

        cond2 = sb("cond2", [128, 8, 2])
        lbd = sm[:, 8:12]
        lb = sm[:, 12:16]
        oml = sm[:, 16:20]
        esink = sm[:, 20:24]
        ss = sm[:, 24:25]
        rs = sm[:, 25:26]
        rstd = sm[:, 26:27]
        ident32 = cstt[:, 0:128]
        XGALL = ["xg%d" % t for t in range(8)]

        def T(i):
            return TS[:, i, :]

        def Tn(i):
            return "T%d" % i

        def dma(eng, out_, in_, r=(), w=(), key=None):
            A(eng, lambda e: e.dma_start(out=out_, in_=in_), r=r, w=w, dma=key)

        dma("sp", pv[:, :], pvec, w=["pv"], key="T:ld0")
        dma("sp", cstt[:, :], cst, w=["cst"], key="T:ld0")
        dma("pool", rows[0:1, :], rowsin[0:1, :], w=["rows0"], key="T:ldp")
        dma("pool", rows[32:33, :], rowsin[1:2, :], w=["rows32"], key="T:ldp")
        dma("sp", bdn[:, :], b_dn, w=["bdn"], key="T:ld0")
        dma("sp", wr[:, :, :], w_router.rearrange("(kc p) e -> p kc e", p=128), w=["wr"], key="T:ld0")
        dma("sp", brbc[:, :], brt.partition_broadcast(128), w=["brbc"], key="T:ld0")
        dma("sp", gfbc[:, :], gfin.partition_broadcast(128), w=["gfbc"], key="T:ld0")
        dma("sp", relbt[:, :], relb, w=["relbt"], key="T:ld0")
        dma("sp", TS[0:32, 0, 0:510], ohb, w=[Tn(0)], key="T:ld0")
        dma("sp", TS[0:8, 1, 0:510], negb, w=[Tn(1)], key="T:ld0")
        A("pool", lambda e: e.memset(onesf[:, :], 1.0), w=["onesf"])
        A("pool", lambda e: e.memset(onesb[:, :], 1.0), w=["onesb"])
        A("pool", lambda e: e.memset(state[:, :, :], 0.0), w=["st%d" % h for h in range(4)])
        A("pool", lambda e: e.memset(statebf[:, :, :], 0.0), w=["sbf%d" % h for h in range(4)])
        A("dve", lambda e: e.tensor_copy(out=identb[:, :], in_=ident32), r=["cst"], w=["identb"])
        for dup in range(2):
            A("act", lambda e, dup=dup: e.activation(out=cond2[:, :, dup], in_=pv[:, PV_C:PV_C + 8], func=AF.Silu), r=["pv"], w=["cond"])
        A("pe", lambda e: e.matmul(PB[5][0:8, 0:510], lhsT=relbt[:, :], rhs=TS[0:32, 0, 0:510], start=True, stop=True),
          r=["relbt", Tn(0)], w=[pbn(5)])
        A("dve", lambda e: e.tensor_tensor(out=TS[0:8, 2, 0:510], in0=PB[5][0:8, 0:510], in1=TS[0:8, 1, 0:510], op=ALU.add),
          r=[pbn(5), Tn(1)], w=[Tn(2)])
        A("act", lambda e: e.activation(out=TS[0:8, 2, 0:510], in_=TS[0:8, 2, 0:510], func=AF.Exp), r=[Tn(2)], w=[Tn(2)])
        for kb in range(2):
            src = TS[0:8, 2, kb * 255:(kb + 1) * 255].unsqueeze(1).to_broadcast([8, 129, 255])
            dma("sp", scr[:, kb, :].rearrange("h (r u) -> h r u", u=255), src, r=[Tn(2)], w=["scr%d" % kb], key="T:scrw")
        for h in range(8):
            for kb in range(2):
                srcap = scr[h, kb, 0:128 * 254].rearrange("(m x) -> m x", x=254)[:, 0:128]
                dma("sp", E[:, kb, h, :], srcap, r=["scr%d" % kb], w=["E%d_%d" % (kb, h)], key="T:scrr")
        ERES = ["E%d_%d" % (kb, h) for kb in range(2) for h in range(8)]
        A("act", lambda e: e.activation(out=esink, in_=pv[:, PV_SINK:PV_SINK + 4], func=AF.Exp), r=["pv"], w=["esink"])
        A("dve", lambda e: e.tensor_tensor(out=lbd, in0=pv[:, PV_LB:PV_LB + 4], in1=pv[:, PV_LB + 4:PV_LB + 8], op=ALU.subtract),
          r=["pv"], w=["lbd"])
        A("act", lambda e: e.activation(out=lb, in_=lbd, func=AF.Sigmoid), r=["lbd"], w=["lb"])
        A("act", lambda e: e.activation(out=oml, in_=lbd, func=AF.Sigmoid, scale=-1.0), r=["lbd"], w=["oml"])
        bu_view = pv[:, PV_BGU:PV_BGU + 512].rearrange("p (q t) -> p q t", t=2)[:, :, 1:2]
        A("dve", lambda e: e.tensor_scalar(bu_view, bu_view, 1.0, None, op0=ALU.add), r=["pv"], w=["pv"])
        for wi in range(6):
            for hf in range(2):
                buf = xg[:, hf * 4:(hf + 1) * 4, :].rearrange("p a (b c) -> p (a b) c", c=512)
                bres = XGALL[hf * 4:(hf + 1) * 4]
                c0 = wi * D + hf * 512
                dma("sp", buf, w_ada[:, c0:c0 + 512].rearrange("(kc p) n -> p kc n", p=128), w=bres, key="ada%d" % hf)
                for jj in range(4):
                    j = hf * 4 + jj
                    for kc in range(8):
                        A("pe", lambda e, j=j, jj=jj, kc=kc, buf=buf: e.matmul(PB[6][:, 2 * j:2 * j + 2], lhsT=buf[:, kc, jj * 128:(jj + 1) * 128],
                                                                               rhs=cond2[:, kc, :], start=(kc == 0), stop=(kc == 7)),
                          r=bres + ["cond"], w=[pbn(6)])
            A("dve", lambda e, wi=wi: e.tensor_tensor(out=modT[:, wi * 8:(wi + 1) * 8], in0=PB[6][:, 0:16:2],
                                                      in1=pv[:, PV_BADA + wi * 8:PV_BADA + (wi + 1) * 8], op=ALU.add),
              r=[pbn(6), "pv"], w=["modT"])
        for (dst, scw, gcol, shw) in ((0, 1, PV_GMIX, 0), (16, 4, PV_GFFN, 3)):
            A("dve", lambda e, dst=dst, scw=scw: e.tensor_scalar(AB[:, dst:dst + 8], modT[:, scw * 8:(scw + 1) * 8], 1.0, None, op0=ALU.add),
              r=["modT"], w=["AB"])
            A("dve", lambda e, dst=dst, gcol=gcol: e.tensor_tensor(out=AB[:, dst:dst + 8], in0=AB[:, dst:dst + 8], in1=pv[:, gcol:gcol + 8], op=ALU.mult),
              r=["AB", "pv"], w=["AB"])
            A("dve", lambda e, dst=dst, shw=shw: e.tensor_copy(out=AB[:, dst + 8:dst + 16], in_=modT[:, shw * 8:(shw + 1) * 8]),
              r=["modT"], w=["AB"])
        for (wi, dstt, dname) in ((2, gt1bc, "gt1bc"), (5, gt2bc, "gt2bc")):
            for j in range(8):
                dt_ = TS[:, 3 + (j % 2), 0:128]
                A("dve", lambda e, dt_=dt_, wi=wi, j=j: e.tensor_scalar(dt_, ident32, modT[:, wi * 8 + j:wi * 8 + j + 1], None, op0=ALU.mult),
                  r=["cst", "modT"], w=[Tn(3 + (j % 2))])
                pbi = 3 + (j // 4)
                A("pe", lambda e, dt_=dt_, pbi=pbi, j=j: e.matmul(PB[pbi][:, (j % 4) * 128:(j % 4 + 1) * 128], lhsT=onesf[:, :], rhs=dt_,
                                                                   start=True, stop=True),
                  r=["onesf", Tn(3 + (j % 2))], w=[pbn(pbi)])
            for half in range(2):
                A("act", lambda e, dstt=dstt, half=half: e.copy(out=dstt[:, half * 512:(half + 1) * 512], in_=PB[3 + half][:, :]),
                  r=[pbn(3 + half)], w=[dname])

        ring_ctr = [0]

        def rres(k):
            return ["ring%d.%d" % (k, jx) for jx in range(4)]

        def ring_slot():
            k = ring_ctr[0] % 3
            ring_ctr[0] += 1
            return k

        def norm_stats(lt):
            xt = xg[:, lt, :]
            A("act", lambda e: e.activation(out=SQ[:, :], in_=xt, func=AF.Square), r=["xg%d" % lt], w=["SQ"])
            A("dve", lambda e: e.reduce_sum(out=ss, in_=SQ[:, :], axis=AX.X), r=["SQ"], w=["ss"])
            A("act", lambda e: e.activation(out=rs, in_=ss, func=AF.Ln, scale=1.0 / D, bias=EPS), r=["ss"], w=["rs"])
            A("act", lambda e: e.activation(out=rstd, in_=rs, func=AF.Exp, scale=-0.5), r=["rs"], w=["rstd"])

        def norm_T(lt, abase, tile):
            xt = xg[:, lt, :]
            norm_stats(lt)
            xb = xn[:, lt % 2, :]
            xr = "xn%d" % (lt % 2)
            A("act", lambda e: e.activation(out=xb, in_=xt, func=AF.Copy, scale=rstd), r=["xg%d" % lt, "rstd"], w=[xr])
            for kc in range(8):
                A("pe", lambda e, kc=kc: e.transpose(out=PBh[:, kc * 128:(kc + 1) * 128], in_=xb[:, kc * 128:(kc + 1) * 128], identity=identb[:, :]),
                  r=[xr, "identb"], w=["pbh"])
            for kc in range(8):
                A("dve", lambda e, kc=kc: e.tensor_scalar(hT[:, kc, tile * 128:(tile + 1) * 128], PBh[:, kc * 128:(kc + 1) * 128],
                                                          AB[:, abase + kc:abase + kc + 1], AB[:, abase + 8 + kc:abase + 9 + kc],
                                                          op0=ALU.mult, op1=ALU.add),
                  r=["pbh", "AB"], w=["hT%d" % tile])

        HT03 = ["hT%d" % t for t in range(4, 8)]
        fm_ctr = [0]

        def fm_chunk(slot, sres, c0, M=128, pofs=0, pb=None, first=True):
            for kc in range(8):
                A("pe", lambda e, kc=kc: e.matmul(PB[pb][pofs:pofs + M, :], lhsT=ring[:, slot, kc, c0:c0 + M], rhs=hT[:, kc, 512:1024],
                                                  start=(kc == 0), stop=(kc == 7)),
                  r=sres + HT03, w=[pbn(pb)])

        def tok_chunk(slot, sres, c0, rowc0, pb):
            for i in range(4):
                for kc in range(8):
                    A("pe", lambda e, i=i, kc=kc: e.matmul(PB[pb][:, i * 128:(i + 1) * 128], lhsT=hT[:, kc, 512 + i * 128:512 + (i + 1) * 128],
                                                           rhs=ring[:, slot, kc, c0:c0 + 128], start=(kc == 0), stop=False),
                      r=sres + ["hT%d" % (4 + i)], w=[pbn(pb)])
                A("pe", lambda e, i=i: e.matmul(PB[pb][:, i * 128:(i + 1) * 128], lhsT=onesb[32:33, :], rhs=rows[32:33, rowc0:rowc0 + 128],
                                                start=False, stop=True),
                  r=["onesb", "rows32"], w=[pbn(pb)])

        def lowp(fn):
            def g(e):
                with nc.allow_low_precision("sum of 4 masked terms with one non-zero"):
                    return fn(e)
            return g

        def v3(ap, c=4):
            return ap.rearrange("p (c q) -> p c q", c=c)

        def phase1(g, sl):
            s = 2 * g + sl
            lt0 = sl * 4
            for i in range(4):
                lt = lt0 + i
                dma("sp", xg[:, lt, :], x[s * 512 + i * 128:s * 512 + (i + 1) * 128, :], w=["xg%d" % lt], key="x%d" % lt)
            kq = ring_slot()
            dma("pool", ring[:, kq, :, 0:512], w_in[:, 0:512].rearrange("(kc p) n -> p kc n", p=128), w=rres(kq), key="ring%d.0" % kq)
            kkv = ring_slot()
            dma("pool", ring[:, kkv, :, 0:256], w_in[:, 512:768].rearrange("(kc p) n -> p kc n", p=128), w=rres(kkv), key="ring%d.0" % kkv)
            for i in range(4):
                norm_T(lt0 + i, 0, 4 + i)
            for c in range(4):
                pb = c % 2
                fm_chunk(kq, rres(kq), c * 64, M=64, pofs=0, pb=pb)
                fm_chunk(kq, rres(kq), (4 + c) * 64, M=64, pofs=64, pb=pb)
                A("act", lambda e, c=c, pb=pb: e.activation(out=qT[:, :, c, :], in_=v3(PB[pb][:, :]), func=AF.Identity,
                                                            bias=pv[:, PV_BQ + c:PV_BQ + c + 1]),
                  r=[pbn(pb), "pv"], w=["qT"])
            fm_chunk(kkv, rres(kkv), 0, pb=2)
            A("act", lambda e: e.activation(out=kT[:, 128:640], in_=PB[2][:, :], func=AF.Identity, bias=pv[:, PV_BIN + 4:PV_BIN + 5]),
              r=[pbn(2), "pv"], w=["kTm"])
            tok_chunk(kkv, rres(kkv), 128, 0, 4)
            A("dve", lambda e: e.tensor_copy(out=vA[:, 1:5, :], in_=v3(PB[4][:, :])), r=[pbn(4)], w=["vAm"])
            def load_head(h):
                k = ring_slot()
                for jx, base in enumerate((768, 1280, 1792, 2304)):
                    dma("pool", ring[:, k, :, jx * 128:(jx + 1) * 128],
                        w_in[:, base + h * 128:base + (h + 1) * 128].rearrange("(kc p) n -> p kc n", p=128),
                        w=["ring%d.%d" % (k, jx)], key="ring%d.%d" % (k, jx))
                return k
            hslots = [load_head(0)]
            def _attention():
                for i in range(4):
                    jblk = s * 4 + i
                    kbs = [1] if jblk == 0 else [0, 1]
                    for gq in range(2):
                        hp0, hp1 = gq * 64, (gq + 1) * 64
                        for n_, kb in enumerate(kbs):
                            kcol = i * 128 + kb * 128
                            pb = n_
                            A("pe", lambda e, kcol=kcol, pb=pb, i=i, hp0=hp0, hp1=hp1: e.matmul(
                                PB[pb][:, :], lhsT=kT[hp0:hp1, kcol:kcol + 128], rhs=qT[hp0:hp1, i, :, :], start=True, stop=True),
                              r=["kTm", "kTc", "qT"], w=[pbn(pb)])
                            A("act", lambda e, pb=pb, kb=kb: e.activation(out=T(kb), in_=PB[pb][:, :], func=AF.Exp, scale=ATT_SCALE),
                              r=[pbn(pb)], w=[Tn(kb)])
                            A("pool", lambda e, kb=kb, gq=gq: e.tensor_tensor(out=v3(BS[:, kb, :]), in0=v3(T(kb)), in1=E[:, kb, gq * 4:(gq + 1) * 4, :],
                                                                                op=ALU.mult),
                              r=[Tn(kb)] + ERES, w=["PT%d" % kb])
                        for n_, kb in enumerate(kbs):
                            A("pe", lambda e, kb=kb, i=i, st_=(n_ == 0), sp_=(n_ == len(kbs) - 1): e.matmul(
                                PB[2][:, :], lhsT=vA[:, i + kb, :], rhs=BS[:, kb, :], start=st_, stop=sp_),
                              r=["vAm", "vAc", "PT%d" % kb], w=[pbn(2)])
                        for n_, kb in enumerate(kbs):
                            A("pe", lambda e, kb=kb, st_=(n_ == 0), sp_=(n_ == len(kbs) - 1): e.matmul(
                                PB[3][:, :], lhsT=onesb[:, :], rhs=BS[:, kb, :], start=st_, stop=sp_),
                              r=["onesb", "PT%d" % kb], w=[pbn(3)])
                        A("dve", lambda e, hp0=hp0, hp1=hp1: e.tensor_tensor(out=v3(TS[hp0:hp1, 2, :]), in0=v3(PB[3][hp0:hp1, :]),
                                                                              in1=esink[hp0:hp1, :].unsqueeze(2).to_broadcast([64, 4, 128]), op=ALU.add),
                          r=[pbn(3), "esink"], w=[Tn(2)])
                        A("dve", lambda e, hp0=hp0, hp1=hp1: e.reciprocal(out=TS[hp0:hp1, 2, :], in_=TS[hp0:hp1, 2, :]), r=[Tn(2)], w=[Tn(2)])
                        A("dve", lambda e, hp0=hp0, hp1=hp1, i=i: e.tensor_tensor(out=aT[hp0:hp1, :, i * 128:(i + 1) * 128], in0=v3(PB[2][hp0:hp1, :]),
                                                                                   in1=v3(TS[hp0:hp1, 2, :]), op=ALU.mult),
                          r=[pbn(2), Tn(2)], w=["aT"])
                A("pool", lambda e: e.tensor_copy(out=kT[:, 0:128], in_=kT[:, 512:640]), r=["kTm"], w=["kTc"])
                A("pool", lambda e: e.tensor_copy(out=vA[:, 0, :], in_=vA[:, 4, :]), r=["vAm"], w=["vAc"])

            def _hgrn():
                LOGF, BC, DQ, EQ, EK, OSB, OSQ, RSB, XS = 8, 9, 10, 11, 12, 3, 4, 13, 14
                QE, QL, KD, KL = 2, 3, 4, 5
                def inproj(h):
                    p_ = h % 2
                    QF, SG, GS = (5, 0)[p_], (6, 1)[p_], (7, 2)[p_]
                    hv = hiT if p_ == 0 else hiT2
                    hn = "hiT" if p_ == 0 else "hiT2"
                    k = hslots[h]
                    sr = ["ring%d.%d" % (k, jx) for jx in range(4)]
                    fm_chunk(k, [sr[0]], 0, pb=5)
                    A("act", lambda e, h=h: e.activation(out=T(QF), in_=PB[5][:, :], func=AF.Silu, bias=pv[:, PV_BIN + 6 + h:PV_BIN + 7 + h]),
                      r=[pbn(5), "pv"], w=[Tn(QF)])
                    fm_chunk(k, [sr[3]], 384, pb=6)
                    A("act", lambda e, h=h: e.activation(out=T(GS), in_=PB[6][:, :], func=AF.Silu, bias=pv[:, PV_BIN + 18 + h:PV_BIN + 19 + h]),
                      r=[pbn(6), "pv"], w=[Tn(GS)])
                    A("pool", lambda e: e.tensor_scalar(T(GS), T(GS), pv[:, PV_NW:PV_NW + 1], 1.0, op0=ALU.mult, op1=ALU.mult),
                      r=[Tn(GS), "pv"], w=[Tn(GS)])
                    fm_chunk(k, [sr[1]], 128, pb=5)
                    A("act", lambda e, h=h: e.activation(out=T(SG), in_=PB[5][:, :], func=AF.Sigmoid, bias=pv[:, PV_BIN + 10 + h:PV_BIN + 11 + h]),
                      r=[pbn(5), "pv"], w=[Tn(SG)])
                    tok_chunk(k, [sr[2]], 256, 128 + h * 128, 4)
                    A("dve", lambda e: e.tensor_copy(out=hv[:, :, :], in_=v3(PB[4][:, :])), r=[pbn(4)], w=[hn])
                    if h + 1 < 4:
                        hslots.append(load_head(h + 1))

                def gates(h):
                    p_ = h % 2
                    QF, SG, GS = (5, 0)[p_], (6, 1)[p_], (7, 2)[p_]
                    hv = hiT if p_ == 0 else hiT2
                    hn = "hiT" if p_ == 0 else "hiT2"
                    A("dve", lambda e, h=h: e.tensor_scalar(T(SG), T(SG), oml[:, h:h + 1], lb[:, h:h + 1], op0=ALU.mult, op1=ALU.add),
                      r=[Tn(SG), "oml", "lb"], w=[Tn(SG)])
                    A("act", lambda e: e.activation(out=T(LOGF), in_=T(SG), func=AF.Ln), r=[Tn(SG)], w=[Tn(LOGF)])
                    A("dve", lambda e: e.tensor_scalar(T(SG), T(SG), -1.0, 1.0, op0=ALU.mult, op1=ALU.add), r=[Tn(SG)], w=[Tn(SG)])
                    for c in range(8):
                        A("dve", lambda e, c=c: e.tensor_tensor_scan(out=TS[:, BC, c * 64:(c + 1) * 64], data0=onesf[:, 0:64],
                                                                     data1=TS[:, LOGF, c * 64:(c + 1) * 64], initial=0.0, op0=ALU.mult, op1=ALU.add),
                          r=[Tn(LOGF), "onesf"], w=[Tn(BC)])
                    A("act", lambda e: e.activation(out=T(LOGF), in_=T(BC), func=AF.Exp), r=[Tn(BC)], w=[Tn(LOGF)])
                    A("pool", lambda e: e.tensor_tensor(out=BS[:, QE, :], in0=T(QF), in1=T(LOGF), op=ALU.mult), r=[Tn(QF), Tn(LOGF)], w=["QE"])
                    A("dve", lambda e: e.tensor_tensor(out=v3(T(DQ), 8), in0=v3(T(BC), 8),
                                                       in1=v3(T(BC), 8)[:, :, 63:64].to_broadcast([128, 8, 64]), op=ALU.subtract),
                      r=[Tn(BC)], w=[Tn(DQ)])
                    A("act", lambda e: e.activation(out=T(EQ), in_=T(DQ), func=AF.Exp, scale=-1.0), r=[Tn(DQ)], w=[Tn(EQ)])
                    A("pool", lambda e: e.tensor_tensor(out=BS[:, KD, :], in0=T(SG), in1=T(EQ), op=ALU.mult), r=[Tn(SG), Tn(EQ)], w=["KD"])
                    for i in range(4):
                        A("pe", lambda e, i=i: e.transpose(out=PBh[:, i * 128:(i + 1) * 128], in_=BS[:, KD, i * 128:(i + 1) * 128], identity=identb[:, :]),
                          r=["KD", "identb"], w=["pbh"])
                    A("act", lambda e: e.copy(out=KDT[:, :, :], in_=v3(PBh[:, 0:512])), r=["pbh"], w=["KDT"])

                def levels_chain(h):
                    p_ = h % 2
                    QF, SG, GS = (5, 0)[p_], (6, 1)[p_], (7, 2)[p_]
                    hv = hiT if p_ == 0 else hiT2
                    hn = "hiT" if p_ == 0 else "hiT2"
                    LBK = (0, 1, 2, 4)
                    for l, (BL, off) in enumerate(((8, 0), (16, 8), (32, 16), (64, 32))):
                        nb = 512 // BL
                        A("dve", lambda e, BL=BL, off=off, nb=nb: e.tensor_tensor(
                            out=T(DQ).rearrange("p (n j) -> p n j", j=BL), in0=T(BC).rearrange("p (n j) -> p n j", j=BL),
                            in1=T(BC).rearrange("p (n j) -> p n j", j=BL)[:, :, off:off + 1].to_broadcast([128, nb, BL]), op=ALU.subtract),
                          r=[Tn(BC)], w=[Tn(DQ)])
                        if l == 0:
                            A("act", lambda e: e.activation(out=T(EQ), in_=T(DQ), func=AF.Exp), r=[Tn(DQ)], w=[Tn(EQ)])
                            A("act", lambda e: e.activation(out=T(EK), in_=T(DQ), func=AF.Exp, scale=-1.0), r=[Tn(DQ)], w=[Tn(EK)])
                        else:
                            A("dve", lambda e: e.tensor_scalar(T(EQ), T(DQ), 0.0, None, op0=ALU.min), r=[Tn(DQ)], w=[Tn(EQ)])
                            A("dve", lambda e: e.tensor_scalar(T(EK), T(DQ), 0.0, None, op0=ALU.max), r=[Tn(DQ)], w=[Tn(EK)])
                            A("act", lambda e: e.activation(out=T(EQ), in_=T(EQ), func=AF.Exp), r=[Tn(EQ)], w=[Tn(EQ)])
                            A("act", lambda e: e.activation(out=T(EK), in_=T(EK), func=AF.Exp, scale=-1.0), r=[Tn(EK)], w=[Tn(EK)])
                        A("pool", lambda e: e.tensor_tensor(out=BS[:, QL, :], in0=T(QF), in1=T(EQ), op=ALU.mult), r=[Tn(QF), Tn(EQ)], w=["QL"])
                        A("dve", lambda e: e.tensor_tensor(out=BS[:, KL, :], in0=T(SG), in1=T(EK), op=ALU.mult), r=[Tn(SG), Tn(EK)], w=["KL"])
                        for i in range(4):
                            A("pe", lambda e, i=i, l=l: e.matmul(PB[LBK[i]][:, l * 128:(l + 1) * 128], lhsT=BS[:, KL, i * 128:(i + 1) * 128],
                                                                  rhs=BS[:, QL, i * 128:(i + 1) * 128], start=True, stop=True),
                              r=["KL", "QL"], w=[pbn(LBK[i])])
                    for i in range(4):
                        A("dve", lambda e, i=i: e.tensor_tensor(out=T(XS), in0=PB[LBK[i]][:, :], in1=cstt[:, 128:640], op=ALU.mult),
                          r=[pbn(LBK[i]), "cst"], w=[Tn(XS)])
                        A("dve", lowp(lambda e, i=i: e.tensor_reduce(out=SC[:, i, :], in_=T(XS).rearrange("p (l t) -> p t l", l=4), axis=AX.X, op=ALU.add)),
                          r=[Tn(XS)], w=["SC%d" % i])
                    A("act", lambda e, h=h: e.copy(out=SBF[:, 0, :], in_=statebf[:, h, :]), r=["sbf%d" % h], w=["SBF0"])
                    for c in range(8):
                        i, hh = divmod(c, 2)
                        pbk = 3 if hh == 0 else 5
                        A("pe", lambda e, i=i, hh=hh, c=c, pbk=pbk: e.matmul(PB[pbk][:, (c // 2) * 128:(c // 2 + 1) * 128],
                                                                             lhsT=KDT[hh * 64:(hh + 1) * 64, i, :],
                                                                             rhs=hv[hh * 64:(hh + 1) * 64, i, :], start=True, stop=True),
                          r=["KDT", hn], w=[pbn(pbk)])
                    for c in range(8):
                        pbk = 3 if c % 2 == 0 else 5
                        A("dve", lambda e, c=c, h=h, pbk=pbk: e.scalar_tensor_tensor(
                            out=state[:, h, :], in0=state[:, h, :], scalar=TS[:, LOGF, c * 64 + 63:c * 64 + 64],
                            in1=PB[pbk][:, (c // 2) * 128:(c // 2 + 1) * 128], op0=ALU.mult, op1=ALU.add),
                          r=["st%d" % h, Tn(LOGF), pbn(pbk)], w=["st%d" % h])
                        if c < 7:
                            A("act", lambda e, c=c, h=h: e.copy(out=SBF[:, c + 1, :], in_=state[:, h, :]), r=["st%d" % h], w=["SBF%d" % (c + 1)])
                        else:
                            A("act", lambda e, h=h: e.copy(out=statebf[:, h, :], in_=state[:, h, :]), r=["st%d" % h], w=["sbf%d" % h])
                    for i in range(4):
                        A("pe", lambda e, i=i: e.matmul(PB[6][:, i * 128:(i + 1) * 128], lhsT=hv[:, i, :], rhs=SC[:, i, :], start=True, stop=False),
                          r=[hn, "SC%d" % i], w=[pbn(6)])
                        for hh in range(2):
                            c = 2 * i + hh
                            A("pe", lambda e, i=i, hh=hh, c=c: e.matmul(PB[6][:, i * 128 + hh * 64:i * 128 + (hh + 1) * 64], lhsT=SBF[:, c, :],
                                                                        rhs=BS[:, QE, c * 64:(c + 1) * 64], start=False, stop=(hh == 1)),
                              r=["SBF%d" % c, "QE"], w=[pbn(6)])

                def norm(h):
                    p_ = h % 2
                    GS = (7, 2)[p_]
                    A("act", lambda e: e.activation(out=T(OSQ), in_=PB[6][:, :], func=AF.Square), r=[pbn(6)], w=[Tn(OSQ)])
                    A("dve", lambda e: e.tensor_copy(out=T(OSB), in_=PB[6][:, :]), r=[pbn(6)], w=[Tn(OSB)])
                    A("pe", lambda e: e.matmul(PB[5][:, :], lhsT=onesf[:, :], rhs=T(OSQ), start=True, stop=True), r=["onesf", Tn(OSQ)], w=[pbn(5)])
                    A("act", lambda e: e.activation(out=T(RSB), in_=PB[5][:, :], func=AF.Ln, scale=1.0 / 128, bias=EPS), r=[pbn(5)], w=[Tn(RSB)])
                    A("act", lambda e: e.activation(out=T(RSB), in_=T(RSB), func=AF.Exp, scale=-0.5), r=[Tn(RSB)], w=[Tn(RSB)])
                    A("dve", lambda e: e.tensor_tensor(out=T(OSB), in0=T(OSB), in1=T(RSB), op=ALU.mult), r=[Tn(OSB), Tn(RSB)], w=[Tn(OSB)])
                    A("pool", lambda e, h=h: e.tensor_tensor(out=mTh[:, h, :], in0=T(OSB), in1=T(GS), op=ALU.mult), r=[Tn(OSB), Tn(GS)], w=["mTh"])

                def lane_h0():
                    inproj(0)
                    gates(0)
                weave(record(_attention), record(lane_h0))
                for h in range(4):
                    if h == 0:
                        inproj(1)
                    levels_chain(h)
                    if h + 1 < 4:
                        def lane_a(h=h):
                            norm(h)
                            if h + 2 < 4:
                                inproj(h + 2)
                        weave(record(lane_a), record(gates, h + 1))
                    else:
                        norm(h)


            _hgrn()
            if "aT" in dbg_out:
                dma("sp", dbg_out["aT"][s], aT[:, :, :], r=["aT"], key="dbgA%d" % s)
                dma("sp", dbg_out["mTh"][s], mTh[:, :, :], r=["mTh"], key="dbgM%d" % s)
            for i in range(4):
                lt = lt0 + i
                for dh in range(2):
                    pb = dh
                    mms = [(aT[:, c, i * 128:(i + 1) * 128], Wd[:, c, dh * 512:(dh + 1) * 512], ["aT", "Wd0"]) for c in range(4)]
                    mms += [(mTh[:, h, i * 128:(i + 1) * 128], Wd[:, 4 + h, dh * 512:(dh + 1) * 512], ["mTh", "Wd1"]) for h in range(4)]
                    mms += [(onesb[0:1, :], rows[0:1, dh * 512:(dh + 1) * 512], ["onesb", "rows0"])]
                    for n_, (l_, r_, res) in enumerate(mms):
                        A("pe", lambda e, l_=l_, r_=r_, pb=pb, st_=(n_ == 0), sp_=(n_ == len(mms) - 1): e.matmul(PB[pb][:, :], lhsT=l_, rhs=r_, start=st_, stop=sp_),
                          r=res, w=[pbn(pb)])
                    A("dve", lambda e, pb=pb, dh=dh: e.tensor_tensor(out=T(3 + dh), in0=PB[pb][:, :], in1=gt1bc[:, dh * 512:(dh + 1) * 512], op=ALU.mult),
                      r=[pbn(pb), "gt1bc"], w=[Tn(3 + dh)])
                    A("pool", lambda e, lt=lt, dh=dh: e.tensor_tensor(out=xg[:, lt, dh * 512:(dh + 1) * 512], in0=T(3 + dh),
                                                                      in1=xg[:, lt, dh * 512:(dh + 1) * 512], op=ALU.add),
                      r=[Tn(3 + dh), "xg%d" % lt], w=["xg%d" % lt])
                if "x1" in dbg_out:
                    dma("sp", dbg_out["x1"][s * 512 + i * 128:s * 512 + (i + 1) * 128, :], xg[:, lt, :], r=["xg%d" % lt], key="dbgX%d" % lt)

        LG, M8, MSK, NM, EX, SMC = rt[:, 0:32], rt[:, 32:40], rt[:, 40:72], rt[:, 72:73], rt[:, 80:112], rt[:, 73:74]

        def router(g, lt):
            xt = xg[:, lt, :]
            norm_T(lt, 16, lt)
            A("act", lambda e: e.activation(out=SQ[:, :], in_=xt, func=AF.Copy, scale=rstd), r=["xg%d" % lt, "rstd"], w=["SQ"])
            for kc in range(8):
                pbi = 2 + kc // 4
                A("pe", lambda e, kc=kc, pbi=pbi: e.transpose(out=PB[pbi][:, (kc % 4) * 128:(kc % 4 + 1) * 128], in_=SQ[:, kc * 128:(kc + 1) * 128],
                                                              identity=ident32), r=["SQ", "cst"], w=[pbn(pbi)])
            H32 = TS[:, 0:2, :].rearrange("p a (b c) -> p (a b) c", c=128)
            for kc in range(8):
                pbi = 2 + kc // 4
                A("act", lambda e, kc=kc, pbi=pbi: e.activation(out=H32[:, kc, :], in_=PB[pbi][:, (kc % 4) * 128:(kc % 4 + 1) * 128], func=AF.Identity,
                                                                scale=AB[:, 16 + kc:17 + kc], bias=AB[:, 24 + kc:25 + kc]),
                  r=[pbn(pbi), "AB"], w=[Tn(kc // 4)])
            for kc in range(8):
                A("pe", lambda e, kc=kc: e.matmul(PB[4][:, 0:NE], lhsT=H32[:, kc, :], rhs=wr[:, kc, :], start=(kc == 0), stop=(kc == 7)),
                  r=[Tn(kc // 4), "wr"], w=[pbn(4)])
            A("dve", lambda e: e.tensor_tensor(out=LG, in0=PB[4][:, 0:NE], in1=brbc[:, :], op=ALU.add), r=[pbn(4), "brbc"], w=["LG"])
            A("dve", lambda e: e.max(out=M8, in_=LG), r=["LG"], w=["M8"])
            A("dve", lambda e: e.tensor_scalar(MSK, LG, M8[:, 3:4], None, op0=ALU.is_ge), r=["LG", "M8"], w=["MSK"])
            A("dve", lambda e: e.tensor_scalar(NM, M8[:, 0:1], -1.0, None, op0=ALU.mult), r=["M8"], w=["NM"])
            A("act", lambda e: e.activation(out=EX, in_=LG, func=AF.Exp, bias=NM), r=["LG", "NM"], w=["EX"])
            A("dve", lambda e: e.tensor_tensor(out=EX, in0=EX, in1=MSK, op=ALU.mult), r=["EX", "MSK"], w=["EX"])
            A("dve", lambda e: e.reduce_sum(out=SMC, in_=EX, axis=AX.X), r=["EX"], w=["SMC"])
            A("dve", lambda e: e.reciprocal(out=SMC, in_=SMC), r=["SMC"], w=["SMC"])
            A("dve", lambda e, lt=lt: e.tensor_scalar(comb[:, lt, :], EX, SMC, None, op0=ALU.mult), r=["EX", "SMC"], w=["comb%d" % lt])
            if "comb" in dbg_out:
                dma("sp", dbg_out["comb"][g * 1024 + lt * 128:g * 1024 + (lt + 1) * 128, :], comb[:, lt, :], r=["comb%d" % lt], key="dbgC%d" % lt)

        def routerB(lt):
            A("pe", lambda e, lt=lt: e.transpose(out=PB[5][0:32, 0:128], in_=comb[:, lt, :], identity=ident32), r=["comb%d" % lt, "cst"], w=[pbn(5)])
            A("act", lambda e: e.copy(out=CT[:, :], in_=PB[5][0:32, 0:128]), r=[pbn(5)], w=["CT"])
            for dh in range(2):
                A("pe", lambda e, dh=dh: e.matmul(PB[dh][:, :], lhsT=CT[:, :], rhs=bdn[:, dh * 512:(dh + 1) * 512], start=True, stop=True),
                  r=["CT", "bdn"], w=[pbn(dh)])
                A("dve", lambda e, dh=dh: e.tensor_tensor(out=T(3 + dh), in0=PB[dh][:, :], in1=gt2bc[:, dh * 512:(dh + 1) * 512], op=ALU.mult),
                  r=[pbn(dh), "gt2bc"], w=[Tn(3 + dh)])
                A("pool", lambda e, lt=lt, dh=dh: e.tensor_tensor(out=xg[:, lt, dh * 512:(dh + 1) * 512], in0=T(3 + dh),
                                                                  in1=xg[:, lt, dh * 512:(dh + 1) * 512], op=ALU.add),
                  r=[Tn(3 + dh), "xg%d" % lt], w=["xg%d" % lt])

        def moe(g):
            pieces = [(e_, pc) for e_ in range(NE) for pc in range(4)]
            slots = {}

            def load_piece(n):
                e_, pc = pieces[n]
                k = ring_slot()
                slots[n] = k
                dma("pool", ring[:, k, :, :], w_gu[e_, :, pc * 512:(pc + 1) * 512].rearrange("(kc p) n -> p kc n", p=128),
                    w=rres(k), key="ring%d.0" % k)

            def load_wd(e_):
                for hf in range(2):
                    dma("pool", Wd[:, hf * 4:(hf + 1) * 4, :], w_dn[e_, hf * 512:(hf + 1) * 512, :].rearrange("(kc p) n -> p kc n", p=128),
                        w=["Wd%d" % hf], key="Wd%d" % hf)

            for n in range(3):
                load_piece(n)
            load_wd(0)
            cnt = 0
            cnt2 = 0
            pend_e = []

            def flush_e():
                while pend_e:
                    U1, T1, ffc, half = pend_e.pop(0)
                    A("dve", lambda e, U1=U1, T1=T1, ffc=ffc, half=half: e.scalar_tensor_tensor(
                        out=actT[:, ffc, half * 512:(half + 1) * 512], in0=T(U1), scalar=8.0, in1=T(T1), op0=ALU.min, op1=ALU.mult),
                      r=[Tn(U1), Tn(T1)], w=["act%d_%d" % (ffc, half)])
            for e_ in range(NE):
                for pc in range(4):
                    n = e_ * 4 + pc
                    k = slots[n]
                    for half in range(2):
                        hres = ["hT%d" % t for t in range(half * 4, half * 4 + 4)]
                        for jj in range(2):
                            ffc = pc * 2 + jj
                            par = cnt % 2
                            cnt += 1
                            pg, pu = par, 2 + par
                            GC, SGM, U1, T1 = 5 + par * 4, 6 + par * 4, 7 + par * 4, 8 + par * 4
                            for kc in range(8):
                                A("pe", lambda e, kc=kc, k=k, jj=jj, pg=pg, half=half: e.matmul(
                                    PB[pg][:, :], lhsT=ring[:, k, kc, jj * 256:jj * 256 + 256:2], rhs=hT[:, kc, half * 512:(half + 1) * 512],
                                    start=(kc == 0), stop=(kc == 7)), r=rres(k) + hres, w=[pbn(pg)])
                            for kc in range(8):
                                A("pe", lambda e, kc=kc, k=k, jj=jj, pu=pu, half=half: e.matmul(
                                    PB[pu][:, :], lhsT=ring[:, k, kc, jj * 256 + 1:jj * 256 + 256:2], rhs=hT[:, kc, half * 512:(half + 1) * 512],
                                    start=(kc == 0), stop=(kc == 7)), r=rres(k) + hres, w=[pbn(pu)])
                            bcol = PV_BGU + e_ * 16 + ffc * 2
                            A("dve", lambda e, pg=pg, GC=GC, bcol=bcol: e.tensor_scalar(T(GC), PB[pg][:, :], pv[:, bcol:bcol + 1], 7.0, op0=ALU.add, op1=ALU.min),
                              r=[pbn(pg), "pv"], w=[Tn(GC)])
                            A("act", lambda e, GC=GC, SGM=SGM: e.activation(out=T(SGM), in_=T(GC), func=AF.Sigmoid, scale=1.702), r=[Tn(GC)], w=[Tn(SGM)])
                            A("dve", lambda e, pu=pu, U1=U1, bcol=bcol: e.tensor_scalar(T(U1), PB[pu][:, :], pv[:, bcol + 1:bcol + 2], -6.0, op0=ALU.add, op1=ALU.max),
                              r=[pbn(pu), "pv"], w=[Tn(U1)])
                            A("pool", lambda e, GC=GC, SGM=SGM, T1=T1: e.tensor_tensor(out=T(T1), in0=T(GC), in1=T(SGM), op=ALU.mult),
                              r=[Tn(GC), Tn(SGM)], w=[Tn(T1)])
                            flush_e()
                            pend_e.append((U1, T1, ffc, half))
                    if n + 3 < len(pieces):
                        load_piece(n + 3)
                for lt in range(8):
                    ares = ["act%d_%d" % (kc, lt // 4) for kc in range(8)]
                    for dh in range(2):
                        py = 4 + cnt2 % 2
                        tm = 3 + cnt2 % 2
                        cnt2 += 1
                        for kc in range(8):
                            A("pe", lambda e, kc=kc, lt=lt, dh=dh, py=py: e.matmul(PB[py][:, :], lhsT=actT[:, kc, lt * 128:(lt + 1) * 128],
                                                                                    rhs=Wd[:, kc, dh * 512:(dh + 1) * 512], start=(kc == 0), stop=(kc == 7)),
                              r=ares + ["Wd0", "Wd1"], w=[pbn(py)])
                        A("dve", lambda e, py=py, tm=tm, dh=dh: e.tensor_tensor(out=T(tm), in0=PB[py][:, :], in1=gt2bc[:, dh * 512:(dh + 1) * 512], op=ALU.mult),
                          r=[pbn(py), "gt2bc"], w=[Tn(tm)])
                        A("dve", lambda e, tm=tm, lt=lt, dh=dh, e_=e_: e.scalar_tensor_tensor(
                            out=xg[:, lt, dh * 512:(dh + 1) * 512], in0=T(tm), scalar=comb[:, lt, e_:e_ + 1], in1=xg[:, lt, dh * 512:(dh + 1) * 512],
                            op0=ALU.mult, op1=ALU.add), r=[Tn(tm), "comb%d" % lt, "xg%d" % lt], w=["xg%d" % lt])
                        if lt == 1 and dh == 1:
                            flush_e()
                    if e_ == NE - 1 and lt >= 2:
                        final(g, [lt - 2])
                if e_ + 1 < NE:
                    load_wd(e_ + 1)

        def final(g, lts=range(8)):
            for lt in lts:
                norm_stats(lt)
                ob = lt % 2
                OUTT = TS[:, 11 + 2 * ob:13 + 2 * ob, :].rearrange("p a b -> p (a b)")
                ores = [Tn(11 + 2 * ob), Tn(12 + 2 * ob)]
                A("dve", lambda e, lt=lt, OUTT=OUTT: e.scalar_tensor_tensor(out=OUTT, in0=xg[:, lt, :], scalar=rstd, in1=gfbc[:, :],
                                                                            op0=ALU.mult, op1=ALU.mult),
                  r=["xg%d" % lt, "rstd", "gfbc"], w=ores)
                dma("sp", out[g * 1024 + lt * 128:g * 1024 + (lt + 1) * 128, :], OUTT, r=ores, key="o%d" % ob)

        for g in range(NGRP):
            for c in range(4):
                for gq in range(2):
                    r0 = (gq * 4 + c) * 64
                    dma("pool", Wd[gq * 64:(gq + 1) * 64, c, :], w_out[r0:r0 + 64, :], w=["Wd0"], key="Wo")
            dma("pool", Wd[:, 4:8, :], w_out[512:1024, :].rearrange("(h p) n -> p h n", p=128), w=["Wd1"], key="Wd1")
            phase1(g, 0)
            phase1(g, 1)
            router(g, 0)
            for lt in range(7):
                weave(record(router, g, lt + 1), record(routerB, lt))
            routerB(7)
            moe(g)
            final(g, [6, 7])
        S.emit(nc)
    return nc


def _t5_bucket(dist):
    n = np.maximum(dist, 0)
    nf = np.maximum(n, 1).astype(np.float32)
    large = 16 + (np.log(nf / np.float32(16)) / np.float32(math.log(128 / 16)) * np.float32(16)).astype(np.int32)
    large = np.minimum(large, 31)
    return np.where(n < 16, n, large)


def _static_tables():
    oh = np.zeros((32, 2, 255), np.float32)
    neg = np.zeros((8, 2, 255), np.float32)
    for u in range(255):
        if u <= 127:
            oh[_t5_bucket(np.array(u)), 1, u] = 1.0
            neg[:, 0, u] = NEG
        else:
            oh[_t5_bucket(np.array(u - 127)), 0, u] = 1.0
            neg[:, 1, u] = NEG
    cst = np.zeros((128, 640), np.float32)
    cst[:, 0:128] = np.eye(128, dtype=np.float32)
    s = np.arange(128)[:, None]
    t = np.arange(128)[None, :]
    cst[:, 128:256] = ((s // 8 == t // 8) & (t >= s)).astype(np.float32)
    for li, BL in enumerate((16, 32, 64)):
        cst[:, 256 + li * 128:384 + li * 128] = ((s // BL == t // BL) & (t % BL >= BL // 2) & (s % BL < BL // 2)).astype(np.float32)
    return oh.reshape(32, 510), neg.reshape(8, 510), cst


def _pack_pvec(b, c, g_mix, b_ada, b_in, g_ffn, hg_lb, hg_norm_w, attn_sinks, b_gate_up):
    pvv = np.zeros((128, NPV), np.float32)
    pvv[:, PV_C:PV_C + 8] = c[b].reshape(8, 128).T
    pvv[:, PV_GMIX:PV_GMIX + 8] = g_mix[0].reshape(8, 128).T
    pvv[:, PV_GFFN:PV_GFFN + 8] = g_ffn[0].reshape(8, 128).T
    pvv[:, PV_BADA:PV_BADA + 48] = b_ada[0].reshape(48, 128).T
    pvv[:, PV_BIN:PV_BIN + 22] = b_in[0].reshape(22, 128).T
    bq = b_in[0][0:512].reshape(8, 64)
    for cc in range(4):
        pvv[0:64, PV_BQ + cc] = bq[cc]
        pvv[64:128, PV_BQ + cc] = bq[4 + cc]
    pvv[:, PV_LB:PV_LB + 8] = hg_lb.reshape(2, 4, 128).transpose(2, 0, 1).reshape(128, 8)
    pvv[:, PV_NW] = hg_norm_w[0]
    pvv[0:64, PV_SINK:PV_SINK + 4] = attn_sinks[0][None, 0:4]
    pvv[64:128, PV_SINK:PV_SINK + 4] = attn_sinks[0][None, 4:8]
    pvv[:, PV_BGU:PV_BGU + 512] = b_gate_up[0].reshape(32, 8, 128, 2).transpose(2, 0, 1, 3).reshape(128, 512)
    return pvv


_NC_CACHE = {}


def run(inputs, SEQ=4096, cores=8, dbg=()):
    f = lambda a: np.ascontiguousarray(np.asarray(a, dtype=np.float32))
    x = f(inputs["x"])
    oh, neg, cst = _static_tables()
    b_in = f(inputs["b_in"])
    rowsin = np.zeros((2, D), np.float32)
    rowsin[0] = f(inputs["b_out"])[0]
    rowsin[1, 0:128] = b_in[0][640:768]
    rowsin[1, 128:640] = b_in[0][1792:2304]
    shared = dict(
        w_ada=f(inputs["w_ada"])[0], w_in=f(inputs["w_in"])[0], rowsin=rowsin, relb=f(inputs["rel_bias"]), ohb=oh, negb=neg, cst=cst,
        w_out=f(inputs["w_out"])[0], w_router=f(inputs["w_router"])[0], brt=f(inputs["b_router"]).reshape(1, NE),
        w_gu=f(inputs["w_gate_up"])[0], w_dn=f(inputs["w_down"])[0], b_dn=f(inputs["b_down"])[0], gfin=f(inputs["g_final"]).reshape(1, D))
    in_maps = []
    for b in range(cores):
        m = dict(shared)
        m["x"] = np.ascontiguousarray(x[b, :SEQ])
        m["pvec"] = _pack_pvec(b, f(inputs["c"]), f(inputs["g_mix"]), f(inputs["b_ada"]), b_in, f(inputs["g_ffn"]), f(inputs["hg_lb"]),
                               f(inputs["hg_norm_w"]), f(inputs["attn_sinks"]), f(inputs["b_gate_up"]))
        in_maps.append(m)
    key = (SEQ, tuple(dbg))
    if key not in _NC_CACHE:
        _NC_CACHE[key] = build(SEQ, dbg)
    res = run_bass_kernel_spmd(_NC_CACHE[key], in_maps, core_ids=list(range(cores)))
    return res


def kernel(**inputs):
    res = run(inputs, SEQ=4096, cores=8)
    return np.stack([np.asarray(r["out"], dtype=np.float32) for r in res.results], axis=0)
```
